# Optimizing a Trainium2 kernel written in Bass

```python
import math
import jax, jax.numpy as jnp
from jax import lax
import numpy as np

D_MODEL = 2048
BATCH = 4
SEQ = 8192
DEPTH = 4

HEAD_DIM = 128
N_HEADS = D_MODEL // HEAD_DIM
A_HEADS = N_HEADS // 2
NSA_HEADS = N_HEADS - A_HEADS
NSA_KV_GROUPS = 2
NSA_REP = NSA_HEADS // NSA_KV_GROUPS
A_WIDTH = A_HEADS * HEAD_DIM
NSA_WIDTH = NSA_HEADS * HEAD_DIM
KV_WIDTH = NSA_KV_GROUPS * HEAD_DIM
N_BRANCH = 3
IN_COLS = 3 * A_WIDTH + NSA_WIDTH + 2 * N_BRANCH * KV_WIDTH + N_BRANCH * NSA_HEADS
D_FF = 5632
DILATED_PATTERNS = ((128, 1), (512, 4), (2048, 16))
BAND = 128
CMP_STRIDE = 16
CMP_LEN = 2 * CMP_STRIDE
CMP_HIDDEN = 512
SLC_LEN = 64
SLC_TOPK = 16
WIN_LEN = 512
NSA_QBLK = 64
N_BUCKETS = 32
MAX_DISTANCE = 2048
FORCE_SCORE = 1e6
NEG_INF = -1e30
EPS = 1e-6

kernel_name = 'hybrid_dilated_nsa_macaron'


def rmsnorm(x, g):
    xf = x.astype(jnp.float32)
    y = xf * lax.rsqrt(jnp.mean(xf * xf, axis=-1, keepdims=True) + EPS)
    return (y * g.astype(jnp.float32)).astype(x.dtype)


def swiglu(x, w1, w3, w2):
    return (jax.nn.silu(x @ w1) * (x @ w3)) @ w2


def rel_bucket(dist):
    n = jnp.maximum(dist, 0)
    max_exact = N_BUCKETS // 2
    nf = jnp.maximum(n, 1).astype(jnp.float32)
    log_b = max_exact + (jnp.log(nf / max_exact) / math.log(MAX_DISTANCE / max_exact) * (N_BUCKETS - max_exact)).astype(jnp.int32)
    return jnp.where(n < max_exact, n, jnp.minimum(log_b, N_BUCKETS - 1))


def masked_probs(logits, mask):
    logits = jnp.where(mask, logits, NEG_INF)
    m = jnp.max(logits, axis=-1, keepdims=True)
    p = jnp.where(mask, jnp.exp(logits - m), 0.0)
    s = jnp.maximum(jnp.sum(p, axis=-1, keepdims=True), 1e-30)
    return p / s, (m + jnp.log(s))[..., 0]


def dilated_pattern(q, k, v, window, dilation, bias_table):
    B, S, H, Dh = q.shape
    span = dilation * BAND
    Sp = -(-S // span) * span
    nb = Sp // span

    def to_sub(t):
        t = jnp.pad(t, ((0, 0), (0, Sp - S), (0, 0), (0, 0)))
        return t.reshape(B, nb, BAND, dilation, H, Dh).transpose(0, 3, 4, 1, 2, 5)

    def with_prev(t):
        prev = jnp.pad(t, ((0, 0), (0, 0), (0, 0), (1, 0), (0, 0), (0, 0)))[:, :, :, :-1]
        return jnp.concatenate([prev, t], axis=4)

    qs = to_sub(q)
    kb = with_prev(to_sub(k))
    vb = with_prev(to_sub(v))
    logits = jnp.einsum('brhnqd,brhnkd->brhnqk', qs, kb).astype(jnp.float32)
    i = jnp.arange(BAND)
    j = jnp.arange(2 * BAND) - BAND
    diff = i[:, None] - j[None, :]
    mask = (diff >= 0) & (diff <= window // dilation)
    mask = mask[None] & ((jnp.arange(nb)[:, None, None] > 0) | (j >= 0)[None, None, :])
    bias = bias_table[rel_bucket(diff * dilation)].astype(jnp.float32).transpose(2, 0, 1)
    probs, lse = masked_probs(logits + bias[None, None, :, None], mask[None, None, None])
    out = jnp.einsum('brhnqk,brhnkd->brhnqd', probs.astype(v.dtype), vb)
    out = out.transpose(0, 3, 4, 1, 2, 5).reshape(B, Sp, H, Dh)[:, :S]
    lse = lse.transpose(0, 3, 4, 1, 2).reshape(B, Sp, H)[:, :S]
    return out, lse


def dilated_attention(q, k, v, bias_table):
    outs, lses = [], []
    for window, dilation in DILATED_PATTERNS:
        o, s = dilated_pattern(q, k, v, window, dilation, bias_table)
        outs.append(o)
        lses.append(s)
    w = jax.nn.softmax(jnp.stack(lses), axis=0)
    return jnp.einsum('pbsh,pbshd->bshd', w.astype(q.dtype), jnp.stack(outs))


def compress(t, pos, w1, w2):
    B, S, G, Dh = t.shape
    ch = t.reshape(B, S // CMP_STRIDE, CMP_STRIDE, G, Dh)
    blocks = jnp.concatenate([ch[:, :-1], ch[:, 1:]], axis=2) + pos[None, None, :, None, :]
    flat = blocks.transpose(0, 1, 3, 2, 4).reshape(B, S // CMP_STRIDE - 1, G, CMP_LEN * Dh)
    return jax.nn.gelu(flat @ w1) @ w2


def nsa_attention(q, kc, vc, ks, vs, kw, vw, gates, bias_table):
    B, S, H, Dh = q.shape
    G, R = NSA_KV_GROUPS, NSA_REP
    n_cmp = kc.shape[1]
    n_slc = S // SLC_LEN
    n_top = min(SLC_TOPK, n_slc)
    cmp_end = jnp.arange(n_cmp) * CMP_STRIDE + CMP_LEN - 1
    c_start = jnp.arange(n_cmp)[:, None] * CMP_STRIDE
    s_start = jnp.arange(n_slc)[None, :] * SLC_LEN
    overlap = ((c_start < s_start + SLC_LEN) & (c_start + CMP_LEN > s_start)).astype(jnp.float32)
    tbl = bias_table.astype(jnp.float32).reshape(N_BUCKETS, G, R).transpose(1, 0, 2)
    ks_t = ks.transpose(0, 2, 1, 3)
    vs_t = vs.transpose(0, 2, 1, 3)
    kw_p = jnp.pad(kw, ((0, 0), (WIN_LEN, 0), (0, 0), (0, 0)))
    vw_p = jnp.pad(vw, ((0, 0), (WIN_LEN, 0), (0, 0), (0, 0)))
    gather_tokens = jax.vmap(jax.vmap(lambda src, ix: src[ix]))
    per_group_bias = jax.vmap(lambda t, b: t[b], in_axes=(0, 1), out_axes=1)

    def block(qi):
        start = qi * NSA_QBLK
        tq = start + jnp.arange(NSA_QBLK)
        qg = lax.dynamic_slice_in_dim(q, start, NSA_QBLK, axis=1).reshape(B, NSA_QBLK, G, R, Dh)
        g = lax.dynamic_slice_in_dim(gates, start, NSA_QBLK, axis=1).reshape(B, NSA_QBLK, G, R, N_BRANCH)
        lc = jnp.einsum('bqgrd,bcgd->bgrqc', qg, kc).astype(jnp.float32)
        pc, _ = masked_probs(lc, cmp_end[None, :] <= tq[:, None])
        o_cmp = jnp.einsum('bgrqc,bcgd->bqgrd', pc.astype(vc.dtype), vc)
        imp = jnp.einsum('bgrqc,cn->bgqn', pc, overlap)
        blk = jnp.arange(n_slc)[None, :]
        cur = (tq // SLC_LEN)[:, None]
        valid = blk * SLC_LEN <= tq[:, None]
        forced = (blk == 0) | (blk == cur) | (blk == cur - 1)
        score = jnp.where(valid & forced, FORCE_SCORE, jnp.where(valid, imp, -1.0))
        _, top = lax.top_k(score, n_top)
        tok = (top[..., None] * SLC_LEN + jnp.arange(SLC_LEN)).reshape(B, G, NSA_QBLK, n_top * SLC_LEN)
        k_sel = gather_tokens(ks_t, tok)
        v_sel = gather_tokens(vs_t, tok)
        dist = tq[None, None, :, None] - tok
        b_sel = per_group_bias(tbl, rel_bucket(dist)).transpose(0, 1, 4, 2, 3)
        ls = jnp.einsum('bqgrd,bgqtd->bgrqt', qg, k_sel).astype(jnp.float32) + b_sel
        ps, _ = masked_probs(ls, (dist >= 0)[:, :, None])
        o_slc = jnp.einsum('bgrqt,bgqtd->bqgrd', ps.astype(vs.dtype), v_sel)
        k_win = lax.dynamic_slice_in_dim(kw_p, start, WIN_LEN + NSA_QBLK, axis=1)
        v_win = lax.dynamic_slice_in_dim(vw_p, start, WIN_LEN + NSA_QBLK, axis=1)
        kpos = start - WIN_LEN + jnp.arange(WIN_LEN + NSA_QBLK)
        dw = tq[:, None] - kpos[None, :]
        wmask = (dw >= 0) & (dw < WIN_LEN) & (kpos >= 0)[None, :]
        b_win = bias_table[rel_bucket(dw)].astype(jnp.float32).reshape(NSA_QBLK, WIN_LEN + NSA_QBLK, G, R).transpose(2, 3, 0, 1)
        lw = jnp.einsum('bqgrd,bkgd->bgrqk', qg, k_win).astype(jnp.float32) + b_win[None]
        pw, _ = masked_probs(lw, wmask)
        o_win = jnp.einsum('bgrqk,bkgd->bqgrd', pw.astype(vw.dtype), v_win)
        out = g[..., 0:1] * o_cmp + g[..., 1:2] * o_slc + g[..., 2:3] * o_win
        return out.reshape(B, NSA_QBLK, H * Dh)

    out = lax.map(block, jnp.arange(S // NSA_QBLK))
    return out.transpose(1, 0, 2, 3).reshape(B, S, H * Dh)


def setup_inputs(seed: int = 0) -> dict:
    key = jax.random.key(seed)
    ks = jax.random.split(key, 24)
    f32 = jnp.float32
    L = DEPTH

    def dense(k, shape, fan_in):
        return jax.random.normal(k, shape, f32) * fan_in ** -0.5

    def gain(k, shape):
        return 1.0 + 0.02 * jax.random.normal(k, shape, f32)

    return {
        'x': jax.random.normal(ks[0], (BATCH, SEQ, D_MODEL), f32),
        'ffn1_norm': gain(ks[1], (L, D_MODEL)),
        'ffn1_w1': dense(ks[2], (L, D_MODEL, D_FF), D_MODEL),
        'ffn1_w3': dense(ks[3], (L, D_MODEL, D_FF), D_MODEL),
        'ffn1_w2': dense(ks[4], (L, D_FF, D_MODEL), D_FF),
        'mix_norm': gain(ks[5], (L, D_MODEL)),
        'w_in': dense(ks[6], (L, D_MODEL, IN_COLS), D_MODEL),
        'gate_bias': 0.1 * jax.random.normal(ks[7], (L, N_BRANCH * NSA_HEADS), f32),
        'q_norm_a': gain(ks[8], (L, HEAD_DIM)),
        'k_norm_a': gain(ks[9], (L, HEAD_DIM)),
        'q_norm_nsa': gain(ks[10], (L, HEAD_DIM)),
        'k_norm_nsa': gain(ks[11], (L, HEAD_DIM)),
        'cmp_pos': 0.2 * jax.random.normal(ks[12], (L, CMP_LEN, HEAD_DIM), f32),
        'cmp_k_w1': dense(ks[13], (L, CMP_LEN * HEAD_DIM, CMP_HIDDEN), CMP_LEN * HEAD_DIM),
        'cmp_k_w2': dense(ks[14], (L, CMP_HIDDEN, HEAD_DIM), CMP_HIDDEN),
        'cmp_v_w1': dense(ks[15], (L, CMP_LEN * HEAD_DIM, CMP_HIDDEN), CMP_LEN * HEAD_DIM),
        'cmp_v_w2': dense(ks[16], (L, CMP_HIDDEN, HEAD_DIM), CMP_HIDDEN),
        'out_norm': gain(ks[17], (L, D_MODEL)),
        'w_out': dense(ks[18], (L, D_MODEL, D_MODEL), D_MODEL),
        'ffn2_norm': gain(ks[19], (L, D_MODEL)),
        'ffn2_w1': dense(ks[20], (L, D_MODEL, D_FF), D_MODEL),
        'ffn2_w3': dense(ks[21], (L, D_MODEL, D_FF), D_MODEL),
        'ffn2_w2': dense(ks[22], (L, D_FF, D_MODEL), D_FF),
        'rel_bias': 0.5 * jax.random.normal(ks[23], (N_BUCKETS, N_HEADS), f32),
    }


def reference(x, ffn1_norm, ffn1_w1, ffn1_w3, ffn1_w2, mix_norm, w_in, gate_bias,
              q_norm_a, k_norm_a, q_norm_nsa, k_norm_nsa, cmp_pos, cmp_k_w1, cmp_k_w2,
              cmp_v_w1, cmp_v_w2, out_norm, w_out, ffn2_norm, ffn2_w1, ffn2_w3, ffn2_w2, rel_bias):
    B, S, _ = x.shape
    scale = HEAD_DIM ** -0.5
    sizes = (A_WIDTH,) * 3 + (NSA_WIDTH,) + (KV_WIDTH,) * (2 * N_BRANCH) + (N_BRANCH * NSA_HEADS,)
    splits = np.cumsum(sizes)[:-1].tolist()

    def heads(t, n):
        return t.reshape(B, S, n, HEAD_DIM)

    for l in range(DEPTH):
        x = x + 0.5 * swiglu(rmsnorm(x, ffn1_norm[l]), ffn1_w1[l], ffn1_w3[l], ffn1_w2[l])
        h = rmsnorm(x, mix_norm[l])
        qa, ka, va, qn, kc, vc, k_s, v_s, k_w, v_w, gl = jnp.split(h @ w_in[l], splits, axis=-1)
        qa = rmsnorm(heads(qa, A_HEADS), q_norm_a[l]) * scale
        ka = rmsnorm(heads(ka, A_HEADS), k_norm_a[l])
        o_a = dilated_attention(qa, ka, heads(va, A_HEADS), rel_bias[:, :A_HEADS]).reshape(B, S, A_WIDTH)
        qn = rmsnorm(heads(qn, NSA_HEADS), q_norm_nsa[l]) * scale
        kc = rmsnorm(compress(heads(kc, NSA_KV_GROUPS), cmp_pos[l], cmp_k_w1[l], cmp_k_w2[l]), k_norm_nsa[l])
        vc = compress(heads(vc, NSA_KV_GROUPS), cmp_pos[l], cmp_v_w1[l], cmp_v_w2[l])
        k_s = rmsnorm(heads(k_s, NSA_KV_GROUPS), k_norm_nsa[l])
        k_w = rmsnorm(heads(k_w, NSA_KV_GROUPS), k_norm_nsa[l])
        gates = jax.nn.sigmoid(gl + gate_bias[l]).reshape(B, S, NSA_HEADS, N_BRANCH)
        o_n = nsa_attention(qn, kc, vc, k_s, heads(v_s, NSA_KV_GROUPS), k_w, heads(v_w, NSA_KV_GROUPS),
                            gates, rel_bias[:, A_HEADS:])
        y = jnp.concatenate([rmsnorm(o_a, out_norm[l, :A_WIDTH]), rmsnorm(o_n, out_norm[l, A_WIDTH:])], axis=-1)
        x = x + y @ w_out[l]
        x = x + 0.5 * swiglu(rmsnorm(x, ffn2_norm[l]), ffn2_w1[l], ffn2_w3[l], ffn2_w2[l])
    return x
```

```python
import contextlib
import numpy as np
import concourse.bass as bass
import concourse.mybir as mybir
from concourse.bass_utils import run_bass_kernel_spmd

F32 = mybir.dt.float32
BF16 = mybir.dt.bfloat16
ALU = mybir.AluOpType
AF = mybir.ActivationFunctionType

D_MODEL = 2048
BATCH = 4
SEQ = 8192
DEPTH = 4
D_FF = 5632
EPS = 1e-6
NCORES = 8
KC = D_MODEL // 128
FC = D_FF // 128


class Buf:
    __slots__ = ("name", "w", "r")

    def __init__(self, name):
        self.name = name
        self.w = None
        self.r = {}


class Eng:
    def __init__(self, name, e, sem):
        self.name = name
        self.e = e
        self.sem = sem
        self.cnt = 0
        self.known = {}


class Sched:
    def __init__(self, nc, stack, n_dma_sems=6):
        self.nc = nc
        self.engs = {}
        for name, e in (("pe", nc.tensor), ("act", nc.scalar), ("dve", nc.vector),
                        ("pool", nc.gpsimd), ("sp", nc.sync)):
            sem = stack.enter_context(nc.semaphore("s_" + name))
            self.engs[name] = Eng(name, e, sem)
        self.dma_sems = {}
        for q in ("sp", "pool", "act"):
            lst = []
            for i in range(n_dma_sems):
                sem = stack.enter_context(nc.semaphore("d_%s%d" % (q, i)))
                lst.append([sem, 0])
            self.dma_sems[q] = [lst, 0]
        self.nwaits = 0
        self.ninst = 0

    def _wait(self, eng, tok):
        sem, val = tok
        if eng.known.get(id(sem), 0) >= val:
            return
        eng.e.wait_ge(sem, val)
        eng.known[id(sem)] = val
        self.nwaits += 1

    def _deps(self, eng, reads, writes, skip_self):
        need = {}

        def add(tok):
            if tok is None:
                return
            sem, val = tok
            if skip_self and sem is eng.sem:
                return
            cur = need.get(id(sem))
            if cur is None or cur[1] < val:
                need[id(sem)] = tok

        for b in reads:
            add(b.w)
        for b in writes:
            add(b.w)
            for t in b.r.values():
                add(t)
        for tok in need.values():
            self._wait(eng, tok)

    def _commit(self, tok, reads, writes):
        sem, val = tok
        for b in reads:
            b.r[id(sem)] = tok
        for b in writes:
            b.w = tok
            b.r = {}

    def op(self, engname, fn, reads=(), writes=()):
        eng = self.engs[engname]
        self._deps(eng, reads, writes, skip_self=(engname == "pe"))
        inst = fn()
        eng.cnt += 1
        inst.then_inc(eng.sem, 1)
        tok = (eng.sem, eng.cnt)
        self._commit(tok, reads, writes)
        self.ninst += 1
        return tok

    def dma(self, q, out, in_, reads=(), writes=(), **kw):
        eng = self.engs[q]
        self._deps(eng, reads, writes, skip_self=False)
        lst, idx = self.dma_sems[q]
        slot = lst[idx]
        self.dma_sems[q][1] = (idx + 1) % len(lst)
        if slot[1] > 0:
            self._wait(eng, (slot[0], slot[1]))
        slot[1] += 16
        eng.e.dma_start(out=out, in_=in_, **kw).then_inc(slot[0], 16)
        tok = (slot[0], slot[1])
        self._commit(tok, reads, writes)
        self.ninst += 1
        return tok

    def barrier(self):
        toks = [(e.sem, e.cnt) for e in self.engs.values() if e.cnt > 0]
        for q in self.dma_sems:
            for slot in self.dma_sems[q][0]:
                if slot[1] > 0:
                    toks.append((slot[0], slot[1]))
        for e in self.engs.values():
            for t in toks:
                if t[0] is not e.sem:
                    self._wait(e, t)

    def finish(self, toks):
        eng = self.engs["sp"]
        for t in toks:
            self._wait(eng, t)


TT = 512
FG = 256
DG = 256


def build_ffn(NT):
    nc = bass.Bass("TRN2", target_bir_lowering=False)
    xT = nc.dram_tensor("xT", [D_MODEL, NT], F32, kind="ExternalInput").ap()
    gn = nc.dram_tensor("gn", [128, KC], F32, kind="ExternalInput").ap()
    w1 = nc.dram_tensor("w1", [D_MODEL, D_FF], F32, kind="ExternalInput").ap()
    w3 = nc.dram_tensor("w3", [D_MODEL, D_FF], F32, kind="ExternalInput").ap()
    w2 = nc.dram_tensor("w2", [D_FF, D_MODEL], F32, kind="ExternalInput").ap()
    yT = nc.dram_tensor("yT", [D_MODEL, NT], F32, kind="ExternalOutput").ap()
    xTv = xT.rearrange("(c p) t -> p c t", p=128)
    yTv = yT.rearrange("(c p) t -> p c t", p=128)
    w1v = w1.rearrange("(c p) f -> p c f", p=128)
    w3v = w3.rearrange("(c p) f -> p c f", p=128)
    w2v = w2.rearrange("(c p) f -> p c f", p=128)
    ntiles = NT // TT
    with contextlib.ExitStack() as st:
        S = Sched(nc, st)
        sb = lambda name, shape, dt: st.enter_context(nc.sbuf_tensor(name, shape, dt))
        ps = lambda name: st.enter_context(nc.psum_tensor(name, [128, 512], F32))
        xt = sb("xt", [128, KC, TT], F32)
        xn = sb("xn", [128, KC, TT], BF16)
        ga = sb("ga", [128, FC, TT], BF16)
        wa = [sb("wa%d" % i, [128, KC, FG], BF16) for i in range(2)]
        wb = [sb("wb%d" % i, [128, KC, FG], BF16) for i in range(2)]
        wc = [sb("wc%d" % i, [128, FC, DG], BF16) for i in range(2)]
        ot = [sb("ot%d" % i, [128, DG // 128, TT], F32) for i in range(2)]
        sl = [sb("sl%d" % i, [128, TT], F32) for i in range(2)]
        rstd = sb("rstd", [128, TT], F32)
        g_sb = sb("g_sb", [128, KC], F32)
        ones = sb("ones", [128, 128], BF16)
        P = [ps("ps%d" % i) for i in range(8)]
        b_xt, b_xn, b_ga, b_rstd, b_g, b_ones = (Buf(n) for n in ("xt", "xn", "ga", "rstd", "g", "ones"))
        b_wa = [Buf("wa0"), Buf("wa1")]
        b_wc = [Buf("wc0"), Buf("wc1")]
        b_ot = [Buf("ot0"), Buf("ot1")]
        b_sl = [Buf("sl0"), Buf("sl1")]
        b_P = [Buf("P%d" % i) for i in range(8)]

        S.dma("sp", g_sb[:], gn, writes=[b_g])
        S.op("dve", lambda: nc.vector.memset(ones[:], 1.0), writes=[b_ones])

        nfg = D_FF // FG
        ndg = D_MODEL // DG
        slabs = []
        for t in range(ntiles):
            for j in range(nfg):
                slabs.append(("a", t, j))
            for j in range(ndg):
                slabs.append(("c", t, j))
        cnt = {"a": 0, "c": 0}
        slab_buf = []
        for kind, t, j in slabs:
            slab_buf.append(cnt[kind] % 2)
            cnt[kind] += 1

        def load_slab(i):
            kind, t, j = slabs[i]
            bi = slab_buf[i]
            if kind == "a":
                S.dma("pool", wa[bi][:], w1v[:, :, j * FG:(j + 1) * FG], writes=[b_wa[bi]])
                S.dma("pool", wb[bi][:], w3v[:, :, j * FG:(j + 1) * FG], writes=[b_wa[bi]])
            else:
                S.dma("pool", wc[bi][:], w2v[:, :, j * DG:(j + 1) * DG], writes=[b_wc[bi]])

        out_toks = []
        load_slab(0)
        si = 0
        pi = 0
        for t in range(ntiles):
            tsl = slice(t * TT, (t + 1) * TT)
            S.dma("sp", xt[:], xTv[:, :, tsl], writes=[b_xt])
            sq = ga[:, 0:KC, :]
            S.op("act", lambda: nc.scalar.activation(out=sq, in_=xt[:], func=AF.Square),
                 reads=[b_xt], writes=[b_ga])
            pss = P[pi % 8]; bp = b_P[pi % 8]; pi += 1

            def mm_ss():
                for c in range(KC):
                    i = nc.tensor.matmul(pss[:], lhsT=ones[:], rhs=ga[:, c, :], start=(c == 0), stop=(c == KC - 1))
                return i
            S.op("pe", mm_ss, reads=[b_ga, b_ones], writes=[bp])
            S.op("dve", lambda: nc.vector.tensor_scalar(out=rstd[:], in0=pss[:], scalar1=1.0 / D_MODEL, scalar2=EPS,
                                                        op0=ALU.mult, op1=ALU.add), reads=[bp], writes=[b_rstd])
            S.op("dve", lambda: nc.vector.reciprocal(out=rstd[:], in_=rstd[:]), reads=[b_rstd], writes=[b_rstd])
            S.op("act", lambda: nc.scalar.activation(out=rstd[:], in_=rstd[:], func=AF.Sqrt),
                 reads=[b_rstd], writes=[b_rstd])

            def mk_xn():
                for c in range(KC):
                    i = nc.vector.scalar_tensor_tensor(out=xn[:, c, :], in0=xt[:, c, :], scalar=g_sb[:, c:c + 1],
                                                       in1=rstd[:], op0=ALU.mult, op1=ALU.mult)
                return i
            S.op("dve", mk_xn, reads=[b_xt, b_rstd, b_g], writes=[b_xn])
            for j in range(nfg):
                bi = slab_buf[si]
                if si + 1 < len(slabs):
                    load_slab(si + 1)
                si += 1
                for q in range(FG // 128):
                    fc = j * (FG // 128) + q
                    p1 = P[pi % 8]; bp1 = b_P[pi % 8]; pi += 1
                    p3 = P[pi % 8]; bp3 = b_P[pi % 8]; pi += 1

                    def mm_up(p1=p1, p3=p3, q=q, bi=bi):
                        for c in range(KC):
                            nc.tensor.matmul(p1[:], lhsT=wa[bi][:, c, q * 128:(q + 1) * 128], rhs=xn[:, c, :],
                                             start=(c == 0), stop=(c == KC - 1))
                        for c in range(KC):
                            i = nc.tensor.matmul(p3[:], lhsT=wb[bi][:, c, q * 128:(q + 1) * 128], rhs=xn[:, c, :],
                                                 start=(c == 0), stop=(c == KC - 1))
                        return i
                    S.op("pe", mm_up, reads=[b_wa[bi], b_xn], writes=[bp1, bp3])
                    s_ = sl[fc % 2]; bs = b_sl[fc % 2]
                    S.op("act", lambda: nc.scalar.activation(out=s_[:], in_=p1[:], func=AF.Silu),
                         reads=[bp1], writes=[bs])
                    S.op("dve", lambda: nc.vector.tensor_tensor(out=ga[:, fc, :], in0=s_[:], in1=p3[:], op=ALU.mult),
                         reads=[bs, bp3], writes=[b_ga])
            for j in range(ndg):
                bi = slab_buf[si]
                if si + 1 < len(slabs):
                    load_slab(si + 1)
                si += 1
                o_ = ot[j % 2]; bo = b_ot[j % 2]
                for q in range(DG // 128):
                    dc = j * (DG // 128) + q
                    po = P[pi % 8]; bpo = b_P[pi % 8]; pi += 1

                    def mm_dn(po=po, q=q, bi=bi):
                        for f in range(FC):
                            i = nc.tensor.matmul(po[:], lhsT=wc[bi][:, f, q * 128:(q + 1) * 128], rhs=ga[:, f, :],
                                                 start=(f == 0), stop=(f == FC - 1))
                        return i
                    S.op("pe", mm_dn, reads=[b_wc[bi], b_ga], writes=[bpo])
                    S.op("dve", lambda: nc.vector.scalar_tensor_tensor(out=o_[:, q, :], in0=po[:], scalar=0.5,
                                                                       in1=xt[:, dc, :], op0=ALU.mult, op1=ALU.add),
                         reads=[bpo, b_xt], writes=[bo])
                dsl = slice(j * (DG // 128), (j + 1) * (DG // 128))
                out_toks.append(S.dma("sp", yTv[:, dsl, tsl], o_[:], reads=[bo]))
        S.finish(out_toks)
        print("ffn program: ops=%d waits=%d" % (S.ninst, S.nwaits))
    return nc


IN_COLS = 5656
HD = 128
FM_CHUNKS = []
for i in range(8):
    FM_CHUNKS.append((0 + 128 * i, i, 0, True))
for i in range(8):
    FM_CHUNKS.append((1024 + 128 * i, 8 + i, 1, False))
for i in range(8):
    FM_CHUNKS.append((3072 + 128 * i, 16 + i, 2, True))
for i in range(2):
    FM_CHUNKS.append((4608 + 128 * i, 24 + i, 3, False))
for i in range(2):
    FM_CHUNKS.append((5120 + 128 * i, 26 + i, 3, False))
for i in range(2):
    FM_CHUNKS.append((4096 + 128 * i, 28 + i, -1, False))
for i in range(2):
    FM_CHUNKS.append((4352 + 128 * i, 30 + i, -1, False))
TM_GROUPS = [(2048, 256, 0), (2304, 256, 256), (2560, 256, 512), (2816, 256, 768),
             (4864, 256, 1024), (5376, 256, 1280)]


def build_mixin(NT):
    nc = bass.Bass("TRN2", target_bir_lowering=False)
    xT = nc.dram_tensor("xT", [D_MODEL, NT], F32, kind="ExternalInput").ap()
    gn = nc.dram_tensor("gn", [128, KC], F32, kind="ExternalInput").ap()
    w_in = nc.dram_tensor("w_in", [D_MODEL, IN_COLS], F32, kind="ExternalInput").ap()
    qkg = nc.dram_tensor("qkg", [128, 4], F32, kind="ExternalInput").ap()
    gbias = nc.dram_tensor("gbias", [1, 24], F32, kind="ExternalInput").ap()
    fmT = nc.dram_tensor("fmT", [32, 128, NT], BF16, kind="ExternalOutput").ap()
    vtok = nc.dram_tensor("vtok", [NT, 1536], BF16, kind="ExternalOutput").ap()
    gates = nc.dram_tensor("gates", [NT, 24], F32, kind="ExternalOutput").ap()
    xTv = xT.rearrange("(c p) t -> p c t", p=128)
    wv = w_in.rearrange("(c p) f -> p c f", p=128)
    ntiles = NT // TT
    with contextlib.ExitStack() as st:
        S = Sched(nc, st)
        sb = lambda name, shape, dt: st.enter_context(nc.sbuf_tensor(name, shape, dt))
        ps = lambda name: st.enter_context(nc.psum_tensor(name, [128, 512], F32))
        xt = sb("xt", [128, KC, TT], F32)
        xn = sb("xn", [128, KC, TT], BF16)
        sq = sb("sq", [128, KC, TT], BF16)
        ws = [sb("ws%d" % i, [128, KC, 256], BF16) for i in range(2)]
        wg = sb("wg", [128, KC, 24], BF16)
        rstd = sb("rstd", [128, TT], F32)
        hsq = [sb("hsq%d" % i, [128, TT], BF16) for i in range(2)]
        hr = [sb("hr%d" % i, [128, TT], F32) for i in range(2)]
        fo = [sb("fo%d" % i, [128, TT], BF16) for i in range(2)]
        to = [sb("to%d" % i, [128, 256], BF16) for i in range(2)]
        go = [sb("go%d" % i, [128, 24], F32) for i in range(2)]
        g_sb = sb("g_sb", [128, KC], F32)
        qkg_sb = sb("qkg_sb", [128, 4], F32)
        gb_sb = sb("gb_sb", [128, 24], F32)
        ones = sb("ones", [128, 128], BF16)
        P = [ps("ps%d" % i) for i in range(8)]
        b_xt, b_xn, b_sq, b_rstd, b_c = (Buf(n) for n in ("xt", "xn", "sq", "rstd", "consts"))
        b_ws = [Buf("ws0"), Buf("ws1")]
        b_wg = Buf("wg")
        b_hsq = [Buf("a"), Buf("b")]
        b_hr = [Buf("a"), Buf("b")]
        b_fo = [Buf("a"), Buf("b")]
        b_to = [Buf("a"), Buf("b")]
        b_go = [Buf("a"), Buf("b")]
        b_P = [Buf("P%d" % i) for i in range(8)]

        S.dma("sp", g_sb[:], gn, writes=[b_c])
        S.dma("sp", qkg_sb[:], qkg, writes=[b_c])
        S.dma("sp", gb_sb[:], gbias.partition_broadcast(128), writes=[b_c])
        S.op("dve", lambda: nc.vector.memset(ones[:], 1.0), writes=[b_c])
        S.dma("pool", wg[:], wv[:, :, 5632:5656], writes=[b_wg])

        fm_slabs = [(FM_CHUNKS[i], FM_CHUNKS[i + 1]) for i in range(0, len(FM_CHUNKS), 2)]
        per_tile = [("fm", s_) for s_ in fm_slabs] + [("tm", g_) for g_ in TM_GROUPS]
        slabs = []
        for t in range(ntiles):
            slabs += per_tile
        def load_slab(i):
            kind, info = slabs[i]
            bi = i % 2
            c0 = info[0][0] if kind == "fm" else info[0]
            S.dma("pool", ws[bi][:], wv[:, :, c0:c0 + 256], writes=[b_ws[bi]])

        out_toks = []
        load_slab(0)
        si = 0
        pi = 0
        ei = 0
        for t in range(ntiles):
            tsl = slice(t * TT, (t + 1) * TT)
            S.dma("sp", xt[:], xTv[:, :, tsl], writes=[b_xt])
            S.op("act", lambda: nc.scalar.activation(out=sq[:], in_=xt[:], func=AF.Square),
                 reads=[b_xt], writes=[b_sq])
            pss = P[pi % 8]; bp = b_P[pi % 8]; pi += 1

            def mm_ss():
                for c in range(KC):
                    i = nc.tensor.matmul(pss[:], lhsT=ones[:], rhs=sq[:, c, :], start=(c == 0), stop=(c == KC - 1))
                return i
            S.op("pe", mm_ss, reads=[b_sq, b_c], writes=[bp])
            S.op("dve", lambda: nc.vector.tensor_scalar(out=rstd[:], in0=pss[:], scalar1=1.0 / D_MODEL, scalar2=EPS,
                                                        op0=ALU.mult, op1=ALU.add), reads=[bp], writes=[b_rstd])
            S.op("dve", lambda: nc.vector.reciprocal(out=rstd[:], in_=rstd[:]), reads=[b_rstd], writes=[b_rstd])
            S.op("act", lambda: nc.scalar.activation(out=rstd[:], in_=rstd[:], func=AF.Sqrt),
                 reads=[b_rstd], writes=[b_rstd])

            def mk_xn():
                for c in range(KC):
                    i = nc.vector.scalar_tensor_tensor(out=xn[:, c, :], in0=xt[:, c, :], scalar=g_sb[:, c:c + 1],
                                                       in1=rstd[:], op0=ALU.mult, op1=ALU.mult)
                return i
            S.op("dve", mk_xn, reads=[b_xt, b_rstd, b_c], writes=[b_xn])

            for kind, info in per_tile:
                bi = si % 2
                if si + 1 < len(slabs):
                    load_slab(si + 1)
                si += 1
                if kind == "fm":
                    for q, (c0, oc, gi, scaled) in enumerate(info):
                        pp = P[pi % 8]; bpp = b_P[pi % 8]; pi += 1

                        def mm(pp=pp, q=q, bi=bi):
                            for c in range(KC):
                                i = nc.tensor.matmul(pp[:], lhsT=ws[bi][:, c, q * 128:(q + 1) * 128], rhs=xn[:, c, :],
                                                     start=(c == 0), stop=(c == KC - 1))
                            return i
                        S.op("pe", mm, reads=[b_ws[bi], b_xn], writes=[bpp])
                        e = ei % 2; ei += 1
                        if gi < 0:
                            S.op("act", lambda: nc.scalar.copy(out=fo[e][:], in_=pp[:]), reads=[bpp], writes=[b_fo[e]])
                        else:
                            S.op("act", lambda: nc.scalar.activation(out=hsq[e][:], in_=pp[:], func=AF.Square),
                                 reads=[bpp], writes=[b_hsq[e]])
                            p2 = P[pi % 8]; bp2 = b_P[pi % 8]; pi += 1
                            S.op("pe", lambda: nc.tensor.matmul(p2[:], lhsT=ones[:], rhs=hsq[e][:], start=True, stop=True),
                                 reads=[b_hsq[e], b_c], writes=[bp2])
                            if scaled:
                                a1, a2 = 1.0, HD * EPS
                            else:
                                a1, a2 = 1.0 / HD, EPS
                            S.op("dve", lambda: nc.vector.tensor_scalar(out=hr[e][:], in0=p2[:], scalar1=a1, scalar2=a2,
                                                                        op0=ALU.mult, op1=ALU.add),
                                 reads=[bp2], writes=[b_hr[e]])
                            S.op("dve", lambda: nc.vector.reciprocal(out=hr[e][:], in_=hr[e][:]),
                                 reads=[b_hr[e]], writes=[b_hr[e]])
                            S.op("act", lambda: nc.scalar.activation(out=hr[e][:], in_=hr[e][:], func=AF.Sqrt),
                                 reads=[b_hr[e]], writes=[b_hr[e]])
                            S.op("dve", lambda: nc.vector.scalar_tensor_tensor(out=fo[e][:], in0=pp[:],
                                                                               scalar=qkg_sb[:, gi:gi + 1], in1=hr[e][:],
                                                                               op0=ALU.mult, op1=ALU.mult),
                                 reads=[bpp, b_hr[e], b_c], writes=[b_fo[e]])
                        out_toks.append(S.dma("sp", fmT[oc, :, tsl], fo[e][:], reads=[b_fo[e]]))
                else:
                    c0, wdt, vc0 = info
                    for sub in range(TT // 128):
                        pp = P[pi % 8]; bpp = b_P[pi % 8]; pi += 1

                        def mm(pp=pp, sub=sub, bi=bi):
                            for c in range(KC):
                                i = nc.tensor.matmul(pp[:, 0:256], lhsT=xn[:, c, sub * 128:(sub + 1) * 128],
                                                     rhs=ws[bi][:, c, :], start=(c == 0), stop=(c == KC - 1))
                            return i
                        S.op("pe", mm, reads=[b_ws[bi], b_xn], writes=[bpp])
                        e = ei % 2; ei += 1
                        S.op("act", lambda: nc.scalar.copy(out=to[e][:], in_=pp[:, 0:256]), reads=[bpp], writes=[b_to[e]])
                        r0 = t * TT + sub * 128
                        out_toks.append(S.dma("sp", vtok[r0:r0 + 128, vc0:vc0 + 256], to[e][:], reads=[b_to[e]]))
            for sub in range(TT // 128):
                pp = P[pi % 8]; bpp = b_P[pi % 8]; pi += 1

                def mm(pp=pp, sub=sub):
                    for c in range(KC):
                        i = nc.tensor.matmul(pp[:, 0:24], lhsT=xn[:, c, sub * 128:(sub + 1) * 128],
                                             rhs=wg[:, c, :], start=(c == 0), stop=(c == KC - 1))
                    return i
                S.op("pe", mm, reads=[b_wg, b_xn], writes=[bpp])
                e = ei % 2; ei += 1
                S.op("dve", lambda: nc.vector.tensor_tensor(out=go[e][:], in0=pp[:, 0:24], in1=gb_sb[:], op=ALU.add),
                     reads=[bpp, b_c], writes=[b_go[e]])
                S.op("act", lambda: nc.scalar.activation(out=go[e][:], in_=go[e][:], func=AF.Sigmoid),
                     reads=[b_go[e]], writes=[b_go[e]])
                r0 = t * TT + sub * 128
                out_toks.append(S.dma("sp", gates[r0:r0 + 128, :], go[e][:], reads=[b_go[e]]))
        S.finish(out_toks)
        print("mixin program: ops=%d waits=%d" % (S.ninst, S.nwaits))
    return nc

NEG = -30000.0
N_BUCKETS = 32


def rel_bucket_np(dist):
    n = np.maximum(dist, 0)
    nf = np.maximum(n, 1).astype(np.float32)
    lb = 16 + (np.log(nf / np.float32(16)) / np.float32(np.log(2048 / 16)) * np.float32(16)).astype(np.int32)
    return np.where(n < 16, n, np.minimum(lb, 31)).astype(np.int64)


def toeplitz_consts(kind):
    if kind == "dil":
        L, base = 3072, 511
    elif kind == "win":
        L, base = 1024, 127
    else:
        L, base = 2048, 127
    dist = np.arange(L) - base
    oh = np.zeros((32, L), np.float32)
    oh[rel_bucket_np(dist), np.arange(L)] = 1.0
    extra = np.zeros((1, L), np.float32)
    if kind == "dil":
        m = ((dist <= 128).astype(np.int64) + ((dist % 4 == 0) & (dist <= 512)) + ((dist % 16 == 0) & (dist <= 2048)))
        m = np.where(dist < 0, 0, m)
        extra[0] = np.where(m > 0, np.log(np.maximum(m, 1)), NEG)
    elif kind == "win":
        extra[0] = np.where((dist >= 0) & (dist < 512), 0.0, NEG)
    else:
        extra[0] = np.where(dist >= 0, 0.0, NEG)
    return oh, extra, L


def gen_toeplitz(nc, S, st, tag, relb_ap, oh_ap, extra_ap, L, Lw, nh, W, b_W, J, b_c, P, b_P):
    sb = lambda name, shape, dt: st.enter_context(nc.sbuf_tensor(tag + name, shape, dt))
    fvd = nc.dram_tensor(tag + "fvd", [nh, L], BF16, kind="Internal")
    relb = sb("relb", [32, nh], F32)
    oh = sb("oh", [32, L], F32)
    ex = sb("ex", [1, L], F32)
    one1 = sb("one1", [1, nh], F32)
    fv = sb("fv", [nh, L], BF16)
    hk = sb("hk", [128, Lw], BF16)
    b_in, b_fv, b_fvd, b_hk = Buf("in"), Buf("fv"), Buf("fvd"), Buf("hk")
    S.dma("sp", relb[:], relb_ap, writes=[b_in])
    S.dma("sp", oh[:], oh_ap, writes=[b_in])
    S.dma("sp", ex[:], extra_ap, writes=[b_in])
    S.op("dve", lambda: nc.vector.memset(one1[:], 1.0), writes=[b_in])
    for c in range(L // 512):
        cs = slice(c * 512, (c + 1) * 512)
        pp, bp = P[c % 2], b_P[c % 2]

        def mm():
            nc.tensor.matmul(pp[0:nh, :], lhsT=relb[:], rhs=oh[:, cs], start=True, stop=False)
            return nc.tensor.matmul(pp[0:nh, :], lhsT=one1[:], rhs=ex[:, cs], start=False, stop=True)
        S.op("pe", mm, reads=[b_in], writes=[bp])
        S.op("dve", lambda: nc.vector.tensor_copy(out=fv[:, cs], in_=pp[0:nh, :]), reads=[bp], writes=[b_fv])
    S.dma("sp", fvd.ap(), fv[:], reads=[b_fv], writes=[b_fvd])
    for h in range(nh):
        src = bass.AP(fvd, h * L, [[1, 128], [1, Lw]])
        S.dma("sp", hk[:], src, reads=[b_fvd], writes=[b_hk])
        c0 = 0
        ci = 0
        while c0 < Lw:
            n = min(512, Lw - c0)
            pp, bp = P[ci % 2], b_P[ci % 2]
            S.op("pe", lambda: nc.tensor.matmul(pp[:, 0:n], lhsT=J[:], rhs=hk[:, c0:c0 + n], start=True, stop=True),
                 reads=[b_hk, b_c], writes=[bp])
            S.op("dve", lambda: nc.vector.tensor_copy(out=W[:, h, c0:c0 + n], in_=pp[:, 0:n]), reads=[bp], writes=[b_W])
            c0 += n
            ci += 1


def host_consts():
    ident = np.eye(128, dtype=np.float32)
    J = np.ascontiguousarray(ident[::-1])
    return ident, J


def build_dilated(SQ=SEQ, NH=4):
    nc = bass.Bass("TRN2", target_bir_lowering=False)
    NB = SQ // 128
    NG = SQ // 512
    qT = nc.dram_tensor("qT", [NH, 128, SQ], BF16, kind="ExternalInput").ap()
    kT = nc.dram_tensor("kT", [NH, 128, SQ], BF16, kind="ExternalInput").ap()
    v = nc.dram_tensor("v", [NH, 128, NB, 128], BF16, kind="ExternalInput").ap()
    relb = nc.dram_tensor("relb", [32, NH], F32, kind="ExternalInput").ap()
    LD = 3072
    LW = 2944
    ohd = nc.dram_tensor("ohd", [32, LD], F32, kind="ExternalInput").ap()
    exd = nc.dram_tensor("exd", [1, LD], F32, kind="ExternalInput").ap()
    identd = nc.dram_tensor("ident", [128, 128], F32, kind="ExternalInput").ap()
    Jd = nc.dram_tensor("J", [128, 128], F32, kind="ExternalInput").ap()
    oT = nc.dram_tensor("oT", [NH, 128, SQ], F32, kind="ExternalOutput").ap()
    with contextlib.ExitStack() as st:
        S = Sched(nc, st)
        sb = lambda name, shape, dt: st.enter_context(nc.sbuf_tensor(name, shape, dt))
        P = [st.enter_context(nc.psum_tensor("ps%d" % i, [128, 512], F32)) for i in range(8)]
        b_P = [Buf("P%d" % i) for i in range(8)]
        b_c = Buf("consts")
        identf = sb("identf", [128, 128], F32)
        identb = sb("identb", [128, 128], BF16)
        Jb = sb("Jb", [128, 128], BF16)
        S.dma("sp", identf[:], identd, writes=[b_c])
        S.dma("pool", identb[:], identd, writes=[b_c])
        S.dma("pool", Jb[:], Jd, writes=[b_c])
        W = sb("W", [128, NH, LW], BF16)
        b_W = Buf("W")
        with contextlib.ExitStack() as st2:
            gen_toeplitz(nc, S, st2, "d_", relb, ohd, exd, LD, LW, NH, W, b_W, Jb, b_c, P, b_P)
        qs = [sb("qs%d" % i, [128, SQ], BF16) for i in range(2)]
        ks = [sb("ks%d" % i, [128, SQ], BF16) for i in range(2)]
        vs = [sb("vs%d" % i, [128, NB, 129], BF16) for i in range(2)]
        b_hd = [Buf("hd0"), Buf("hd1")]
        for i in range(2):
            S.op("dve", lambda: nc.vector.memset(vs[i][:, :, 128:129], 1.0), writes=[b_hd[i]])
        NPT = 3
        pt = [sb("pt%d" % i, [128, 512], BF16) for i in range(NPT)]
        b_pt = [Buf("pt%d" % i) for i in range(NPT)]
        osb = [sb("osb%d" % i, [128, 4, 128], F32) for i in range(2)]
        b_osb = [Buf("osb0"), Buf("osb1")]
        rd = [sb("rd%d" % i, [128, 4], F32) for i in range(2)]
        b_rd = [Buf("rd0"), Buf("rd1")]
        ost = [sb("ost%d" % i, [128, 512], F32) for i in range(2)]
        b_ost = [Buf("ost0"), Buf("ost1")]
        ST_BANKS = [0, 1, 2]
        ACC = [(3, 4), (5, 6)]
        TRB = 7

        def load_head(h):
            i = h % 2
            S.dma("sp", qs[i][:], qT[h], writes=[b_hd[i]])
            S.dma("sp", ks[i][:], kT[h], writes=[b_hd[i]])
            S.dma("sp", vs[i][:, :, 0:128], v[h], writes=[b_hd[i]])

        units = []
        for h in range(NH):
            for G in range(NG):
                kbs = list(range(max(0, 4 * G - 16), 4 * G + 4))
                for kb in kbs:
                    units.append((h, G, kb, kb == kbs[0], kb == kbs[-1]))
        out_toks = []
        gcount = [0]

        def emit_st(u, ui):
            h, G, kb, first, last = u
            i = h % 2
            bank = ST_BANKS[ui % 3]
            d = 4 * G - kb

            def mm():
                nc.tensor.matmul(P[bank][:], lhsT=ks[i][:, kb * 128:(kb + 1) * 128], rhs=qs[i][:, G * 512:(G + 1) * 512],
                                 start=True, stop=False)
                return nc.tensor.matmul(P[bank][:], lhsT=identb[:], rhs=W[:, h, 128 * (d + 3):128 * (d + 3) + 512],
                                        start=False, stop=True)
            S.op("pe", mm, reads=[b_hd[i], b_W, b_c], writes=[b_P[bank]])

        def emit_exp_pv(u, ui):
            h, G, kb, first, last = u
            i = h % 2
            bank = ST_BANKS[ui % 3]
            e = ui % NPT
            S.op("act", lambda: nc.scalar.activation(out=pt[e][:], in_=P[bank][:], func=AF.Exp),
                 reads=[b_P[bank]], writes=[b_pt[e]])
            gi = (h * NG + G) % 2
            a0, a1 = ACC[gi]

            def mm():
                inst = None
                for qi in range(4):
                    db = 4 * G + qi - kb
                    if db < 0 or db > 16:
                        continue
                    kfirst = max(0, 4 * G + qi - 16)
                    bank_a = a0 if qi < 2 else a1
                    inst = nc.tensor.matmul(P[bank_a][:, (qi % 2) * 129:(qi % 2) * 129 + 129],
                                            lhsT=pt[e][:, qi * 128:(qi + 1) * 128], rhs=vs[i][:, kb, :],
                                            start=(kb == kfirst and qi % 2 == 0), stop=(db == 0), skip_group_check=True)
                return inst
            S.op("pe", mm, reads=[b_pt[e], b_hd[i]], writes=[b_P[a0], b_P[a1]])

        def emit_norm(u):
            h, G, kb, first, last = u
            gi = (h * NG + G) % 2
            a0, a1 = ACC[gi]
            o_, r_ = osb[gi], rd[gi]

            def f1():
                inst = None
                for qi in range(4):
                    bank_a = a0 if qi < 2 else a1
                    c = (qi % 2) * 129
                    inst = nc.vector.reciprocal(out=r_[:, qi:qi + 1], in_=P[bank_a][:, c + 128:c + 129])
                return inst
            S.op("dve", f1, reads=[b_P[a0], b_P[a1]], writes=[b_rd[gi]])

            def f2():
                inst = None
                for qi in range(4):
                    bank_a = a0 if qi < 2 else a1
                    c = (qi % 2) * 129
                    inst = nc.vector.tensor_scalar(out=o_[:, qi, :], in0=P[bank_a][:, c:c + 128], scalar1=r_[:, qi:qi + 1],
                                                   scalar2=None, op0=ALU.mult)
                return inst
            S.op("dve", f2, reads=[b_P[a0], b_P[a1], b_rd[gi]], writes=[b_osb[gi]])

        def emit_tr(u):
            h, G, kb, first, last = u
            gi = (h * NG + G) % 2
            o_ = osb[gi]

            def tr():
                inst = None
                for qi in range(4):
                    inst = nc.tensor.transpose(out=P[TRB][:, qi * 128:(qi + 1) * 128], in_=o_[:, qi, :], identity=identf[:])
                return inst
            S.op("pe", tr, reads=[b_osb[gi], b_c], writes=[b_P[TRB]])
            S.op("act", lambda: nc.scalar.copy(out=ost[gi][:], in_=P[TRB][:]), reads=[b_P[TRB]], writes=[b_ost[gi]])
            out_toks.append(S.dma("sp", oT[h, :, G * 512:(G + 1) * 512], ost[gi][:], reads=[b_ost[gi]]))

        pending = []
        load_head(0)
        emit_st(units[0], 0)
        for ui, u in enumerate(units):
            if ui + 1 < len(units):
                emit_st(units[ui + 1], ui + 1)
            emit_exp_pv(u, ui)
            if u[1] == 0 and u[3] and u[0] + 1 < NH:
                load_head(u[0] + 1)
            if u[4]:
                emit_norm(u)
                pending.append([2, u])
            np_ = []
            for p_ in pending:
                p_[0] -= 1
                if p_[0] <= 0:
                    emit_tr(p_[1])
                else:
                    np_.append(p_)
            pending = np_
        for p_ in pending:
            emit_tr(p_[1])
        S.finish(out_toks)
        print("dilated program: ops=%d waits=%d" % (S.ninst, S.nwaits))
    return nc


def nsa_consts(SQ=SEQ):
    ncmp = SQ // 16
    ovl = np.zeros((ncmp, 128), np.float32)
    c = np.arange(ncmp)[:, None] * 16
    n = np.arange(128)[None, :] * 64
    ovl[:] = ((c < n + 64) & (c + 32 > n)).astype(np.float32)
    ovl[ncmp - 1] = 0.0
    ovl = np.ascontiguousarray(ovl.reshape(ncmp // 128, 128, 128).transpose(1, 0, 2))
    j = np.arange(128)[:, None, None]
    o = np.arange(16)[None, :, None]
    i = np.arange(128)[None, None, :]
    mcmp = np.where(16 * j + 31 <= 128 * o + i, 0.0, NEG).astype(np.float32)
    rw = np.zeros((128, SQ), np.float32)
    rw[np.arange(SQ) // 64, np.arange(SQ)] = -NEG
    ii = np.arange(128)[:, None]
    rel = np.arange(256)[None, :] - 126
    ci = (ii >= 64).astype(np.int64)
    valid = rel <= ci
    forced = (rel == ci) | (rel == ci - 1)
    vm = (valid & ~forced).astype(np.float32)
    am = np.where(forced, 1e6, np.where(valid, 0.0, -1.0)).astype(np.float32)
    return ovl, mcmp, rw, vm, am


def build_nsa(SQ=SEQ):
    nc = bass.Bass("TRN2", target_bir_lowering=False)
    NB = SQ // 128
    NCH = SQ // 512
    dt_in = lambda name, shape, dt: nc.dram_tensor(name, shape, dt, kind="ExternalInput").ap()
    qT = dt_in("qT", [4, 128, SQ], BF16)
    ksT = dt_in("ksT", [128, SQ], BF16)
    kwT = dt_in("kwT", [128, SQ], BF16)
    vsd = dt_in("vs", [128, NB, 128], BF16)
    vwd = dt_in("vw", [128, NB, 128], BF16)
    kcr = dt_in("kcr", [128, SQ], BF16)
    vcr = dt_in("vcr", [128, SQ], BF16)
    gat = dt_in("gat", [128, NB, 12], F32)
    posT = dt_in("posT", [128, 32], F32)
    kw1 = dt_in("kw1", [4096, 512], F32)
    kw2 = dt_in("kw2", [512, 128], F32)
    vw1 = dt_in("vw1", [4096, 512], F32)
    vw2 = dt_in("vw2", [512, 128], F32)
    kg = dt_in("kg", [128, 1], F32)
    relb = dt_in("relb", [32, 4], F32)
    LWN, LWW = 1024, 640
    LSN, LSW = 2048, 1792
    ohw = dt_in("ohw", [32, LWN], F32)
    exw = dt_in("exw", [1, LWN], F32)
    ohs = dt_in("ohs", [32, LSN], F32)
    exs = dt_in("exs", [1, LSN], F32)
    identd = dt_in("ident", [128, 128], F32)
    Jd = dt_in("J", [128, 128], F32)
    ovld = dt_in("ovl", [128, SQ // 2048, 128], F32)
    mcmpd = dt_in("mcmp", [128, 16, 128], F32)
    rwd = dt_in("rw", [128, SQ], F32)
    vmd = dt_in("vm", [128, 256], F32)
    amd = dt_in("am", [128, 256], F32)
    oT = nc.dram_tensor("oT", [4, 128, SQ], F32, kind="ExternalOutput").ap()
    NCMP = SQ // 16
    NCB = NCMP // 128
    with contextlib.ExitStack() as st:
        S = Sched(nc, st)
        sb = lambda name, shape, dt: st.enter_context(nc.sbuf_tensor(name, shape, dt))
        P = [st.enter_context(nc.psum_tensor("ps%d" % i, [128, 512], F32)) for i in range(8)]
        b_P = [Buf("P%d" % i) for i in range(8)]
        b_c = Buf("consts")
        identf = sb("identf", [128, 128], F32)
        identb = sb("identb", [128, 128], BF16)
        Jb = sb("Jb", [128, 128], BF16)
        ones = sb("ones", [128, 128], BF16)
        ovl = sb("ovl_sb", [128, SQ // 2048, 128], BF16)
        mcmp = sb("mcmp_sb", [128, 16, 128], BF16)
        rw = sb("rw_sb", [128, SQ], BF16)
        vm = sb("vm_sb", [128, 256], F32)
        am = sb("am_sb", [128, 256], F32)
        gts = sb("gts", [128, NB, 12], F32)
        kg_sb = sb("kg_sb", [128, 1], F32)
        S.dma("sp", identf[:], identd, writes=[b_c])
        S.dma("pool", identb[:], identd, writes=[b_c])
        S.dma("pool", Jb[:], Jd, writes=[b_c])
        S.dma("pool", ovl[:], ovld, writes=[b_c])
        S.dma("pool", mcmp[:], mcmpd, writes=[b_c])
        S.dma("pool", rw[:], rwd[:, 0:SQ], writes=[b_c])
        S.dma("sp", vm[:], vmd, writes=[b_c])
        S.dma("sp", am[:], amd, writes=[b_c])
        S.dma("sp", gts[:], gat, writes=[b_c])
        S.dma("sp", kg_sb[:], kg, writes=[b_c])
        S.op("dve", lambda: nc.vector.memset(ones[:], 1.0), writes=[b_c])
        Ww = sb("Ww", [128, 4, LWW], BF16)
        Ws = sb("Ws", [128, 4, LSW], BF16)
        b_W = Buf("W")
        with contextlib.ExitStack() as st2:
            gen_toeplitz(nc, S, st2, "w_", relb, ohw, exw, LWN, LWW, 4, Ww, b_W, Jb, b_c, P, b_P)
        S.barrier()
        with contextlib.ExitStack() as st2:
            gen_toeplitz(nc, S, st2, "s_", relb, ohs, exs, LSN, LSW, 4, Ws, b_W, Jb, b_c, P, b_P)
        S.barrier()
        kcT = sb("kcT", [128, NCMP], BF16)
        vca = sb("vca", [128, NCB, 129], BF16)
        b_kc = Buf("kc")
        S.op("dve", lambda: nc.vector.memset(vca[:, :, 128:129], 1.0), writes=[b_kc])
        with contextlib.ExitStack() as st2:
            sb2 = lambda name, shape, dt: st2.enter_context(nc.sbuf_tensor(name, shape, dt))
            raw = sb2("raw", [128, SQ], BF16)
            D = sb2("D", [128, 32, NCMP], BF16)
            W1 = sb2("W1", [128, 32, 512], BF16)
            W2 = sb2("W2", [128, 4, 128], BF16)
            pos_sb = sb2("pos_sb", [128, 32], F32)
            hx = sb2("hx", [128, NCMP], F32)
            hu = sb2("hu", [128, NCMP], F32)
            hid = sb2("hid", [128, 4, NCMP], BF16)
            ksq = sb2("ksq", [128, NCMP], BF16)
            krs = sb2("krs", [128, NCMP], F32)
            b_raw, b_D, b_W1, b_W2, b_pos, b_h, b_hid, b_k = (Buf(n) for n in "raw D W1 W2 pos h hid k".split())
            S.dma("sp", pos_sb[:], posT, writes=[b_pos])
            for which in range(2):
                rawd, w1d, w2d = (kcr, kw1, kw2) if which == 0 else (vcr, vw1, vw2)
                S.dma("sp", raw[:], rawd, writes=[b_raw])
                S.dma("pool", W1[:], w1d.rearrange("(j d) h -> d j h", d=128), writes=[b_W1])
                S.dma("pool", W2[:], w2d.rearrange("(c p) d -> p c d", p=128), writes=[b_W2])
                S.op("dve", lambda: nc.vector.memset(D[:], 0.0), writes=[b_D])

                def mkD():
                    inst = None
                    for j in range(32):
                        inst = nc.vector.tensor_scalar(out=D[:, j, 0:NCMP - 1], in0=raw[:, j:j + 16 * (NCMP - 2) + 1:16],
                                                       scalar1=pos_sb[:, j:j + 1], scalar2=None, op0=ALU.add)
                    return inst
                S.op("dve", mkD, reads=[b_raw, b_pos], writes=[b_D])
                for hc in range(4):
                    pp, bp = P[hc % 2], b_P[hc % 2]

                    def mm():
                        inst = None
                        for j in range(32):
                            inst = nc.tensor.matmul(pp[:, 0:NCMP], lhsT=W1[:, j, hc * 128:(hc + 1) * 128], rhs=D[:, j, :],
                                                    start=(j == 0), stop=(j == 31))
                        return inst
                    S.op("pe", mm, reads=[b_W1, b_D], writes=[bp])
                    S.op("act", lambda: nc.scalar.activation(out=hu[:], in_=pp[:, 0:NCMP], func=AF.Square), reads=[bp], writes=[b_h])
                    S.op("dve", lambda: nc.vector.tensor_scalar(out=hu[:], in0=hu[:], scalar1=0.044715, scalar2=1.0,
                                                                op0=ALU.mult, op1=ALU.add), reads=[b_h], writes=[b_h])
                    S.op("dve", lambda: nc.vector.tensor_tensor(out=hu[:], in0=hu[:], in1=pp[:, 0:NCMP], op=ALU.mult),
                         reads=[b_h, bp], writes=[b_h])
                    S.op("act", lambda: nc.scalar.activation(out=hu[:], in_=hu[:], func=AF.Sigmoid, scale=1.5957691216),
                         reads=[b_h], writes=[b_h])
                    S.op("dve", lambda: nc.vector.tensor_tensor(out=hid[:, hc, :], in0=hu[:], in1=pp[:, 0:NCMP], op=ALU.mult),
                         reads=[b_h, bp], writes=[b_hid])
                if which == 0:
                    pp, bp = P[2], b_P[2]

                    def mm():
                        inst = None
                        for hc in range(4):
                            inst = nc.tensor.matmul(pp[:, 0:NCMP], lhsT=W2[:, hc, :], rhs=hid[:, hc, :], start=(hc == 0), stop=(hc == 3))
                        return inst
                    S.op("pe", mm, reads=[b_W2, b_hid], writes=[bp])
                    S.op("act", lambda: nc.scalar.activation(out=ksq[:], in_=pp[:, 0:NCMP], func=AF.Square), reads=[bp], writes=[b_k])
                    p2, bp2 = P[3], b_P[3]
                    S.op("pe", lambda: nc.tensor.matmul(p2[:, 0:NCMP], lhsT=ones[:], rhs=ksq[:], start=True, stop=True),
                         reads=[b_k, b_c], writes=[bp2])
                    S.op("dve", lambda: nc.vector.tensor_scalar(out=krs[:], in0=p2[:, 0:NCMP], scalar1=1.0 / HD, scalar2=EPS,
                                                                op0=ALU.mult, op1=ALU.add), reads=[bp2], writes=[b_k])
                    S.op("dve", lambda: nc.vector.reciprocal(out=krs[:], in_=krs[:]), reads=[b_k], writes=[b_k])
                    S.op("act", lambda: nc.scalar.activation(out=krs[:], in_=krs[:], func=AF.Sqrt), reads=[b_k], writes=[b_k])
                    S.op("dve", lambda: nc.vector.scalar_tensor_tensor(out=kcT[:], in0=pp[:, 0:NCMP], scalar=kg_sb[:, 0:1], in1=krs[:],
                                                                       op0=ALU.mult, op1=ALU.mult),
                         reads=[bp, b_k, b_c], writes=[b_kc])
                else:
                    for cb in range(NCB):
                        pp, bp = P[2 + cb % 2], b_P[2 + cb % 2]

                        def mm():
                            inst = None
                            for hc in range(4):
                                inst = nc.tensor.matmul(pp[:, 0:128], lhsT=hid[:, hc, cb * 128:(cb + 1) * 128], rhs=W2[:, hc, :],
                                                        start=(hc == 0), stop=(hc == 3))
                            return inst
                        S.op("pe", mm, reads=[b_W2, b_hid], writes=[bp])
                        S.op("dve", lambda: nc.vector.tensor_copy(out=vca[:, cb, 0:128], in_=pp[:, 0:128]),
                             reads=[bp], writes=[b_kc])
        S.barrier()
        ks_sb = sb("ks_sb", [128, SQ], BF16)
        kw_sb = sb("kw_sb", [128, SQ], BF16)
        vsa = sb("vsa", [128, NB, 129], BF16)
        vwa = sb("vwa", [128, NB, 129], BF16)
        b_kv = Buf("kv")
        S.dma("sp", ks_sb[:], ksT, writes=[b_kv])
        S.dma("sp", kw_sb[:], kwT, writes=[b_kv])
        S.dma("sp", vsa[:, :, 0:128], vsd, writes=[b_kv])
        S.dma("sp", vwa[:, :, 0:128], vwd, writes=[b_kv])
        S.op("dve", lambda: nc.vector.memset(vsa[:, :, 128:129], 1.0), writes=[b_kv])
        S.op("dve", lambda: nc.vector.memset(vwa[:, :, 128:129], 1.0), writes=[b_kv])
        qb_ = [sb("qb%d" % i, [128, 4, 512], BF16) for i in range(2)]
        b_q = [Buf("q0"), Buf("q1")]
        NPT = 3
        pt = [sb("pt%d" % i, [128, 512], BF16) for i in range(NPT)]
        b_pt = [Buf("pt%d" % i) for i in range(NPT)]
        osb = [sb("osb%d" % i, [128, 4, 128], F32) for i in range(2)]
        b_osb = [Buf("osb0"), Buf("osb1")]
        negT = [sb("negT%d" % i, [128, 128], BF16) for i in range(2)]
        b_neg = [Buf("n0"), Buf("n1")]
        cf = sb("cf", [128, 4], F32)
        b_cf = Buf("cf")
        imp = sb("imp", [128, 128], F32)
        sc = sb("sc", [128, 128], F32)
        sc2 = sb("sc2", [128, 128], F32)
        m8a = sb("m8a", [128, 8], F32)
        m8b = sb("m8b", [128, 8], F32)
        ngm = sb("ngm", [128, 128], F32)
        b_sel = Buf("sel")
        ost = [sb("ost%d" % i, [128, 4, 128], F32) for i in range(2)]
        b_ost = [Buf("ost0"), Buf("ost1")]
        STB = [0, 1]
        ACC_X = (2, 3)
        ACC_S = (4, 5)
        UB = 6
        TRB = 7
        out_toks = []
        uctr = [0]

        def q_tile(qb):
            return qb_[(qb // 4) % 2][:, :, (qb % 4) * 128:(qb % 4) * 128 + 128]

        def load_q(ch):
            S.dma("sp", qb_[ch % 2][:], qT[:, :, ch * 512:(ch + 1) * 512].rearrange("r d q -> d r q"), writes=[b_q[ch % 2]])

        def emit_st(u):
            kind, qb, kb, first, last = u[:5]
            ui = u[5]
            bank = STB[ui % 2]
            bq = b_q[(qb // 4) % 2]
            qt = q_tile(qb)
            P3 = P[bank][:].rearrange("p (r q) -> p r q", r=4)
            if kind == "c":
                diag = (kb == qb // 16)

                def mm():
                    inst = nc.tensor.matmul(P3, lhsT=kcT[:, kb * 128:(kb + 1) * 128], rhs=qt, start=True, stop=True)
                    if diag:
                        for r in range(4):
                            inst = nc.tensor.matmul(P[bank][:, r * 128:(r + 1) * 128], lhsT=identb[:], rhs=mcmp[:, qb % 16, :],
                                                    start=False, stop=(r == 3), skip_group_check=True)
                    return inst
                S.op("pe", mm, reads=[b_kc, bq, b_c], writes=[b_P[bank]])
            elif kind == "s":
                d = min(qb - kb, 13)
                nb_ = negT[qb % 2]

                def mm():
                    nc.tensor.matmul(P3, lhsT=ks_sb[:, kb * 128:(kb + 1) * 128], rhs=qt, start=True, stop=True)
                    nc.tensor.matmul(P3, lhsT=identb[:], rhs=Ws[:, :, d * 128:(d + 1) * 128], start=False, stop=False,
                                     skip_group_check=True)
                    inst = None
                    for r in range(4):
                        inst = nc.tensor.matmul(P[bank][:, r * 128:(r + 1) * 128], lhsT=rw[:, kb * 128:(kb + 1) * 128], rhs=nb_[:],
                                                start=False, stop=(r == 3), skip_group_check=True)
                    return inst
                S.op("pe", mm, reads=[b_kv, bq, b_c, b_W, b_neg[qb % 2]], writes=[b_P[bank]])
            else:
                d = qb - kb

                def mm():
                    nc.tensor.matmul(P3, lhsT=kw_sb[:, kb * 128:(kb + 1) * 128], rhs=qt, start=True, stop=True)
                    return nc.tensor.matmul(P3, lhsT=identb[:], rhs=Ww[:, :, d * 128:(d + 1) * 128], start=False, stop=True,
                                            skip_group_check=True)
                S.op("pe", mm, reads=[b_kv, bq, b_c, b_W], writes=[b_P[bank]])

        def acc_ap(banks, r):
            return P[banks[r // 2]][:, (r % 2) * 129:(r % 2) * 129 + 129]

        def emit_exp_pv(u):
            kind, qb, kb, first, last = u[:5]
            ui = u[5]
            bank = STB[ui % 2]
            e = ui % NPT
            S.op("act", lambda: nc.scalar.activation(out=pt[e][:], in_=P[bank][:], func=AF.Exp),
                 reads=[b_P[bank]], writes=[b_pt[e]])
            if kind == "c":
                banks, vv, bv = ACC_X, vca, b_kc
            elif kind == "s":
                banks, vv, bv = ACC_S, vsa, b_kv
            else:
                banks, vv, bv = ACC_X, vwa, b_kv

            def mm():
                inst = None
                for r in range(4):
                    inst = nc.tensor.matmul(acc_ap(banks, r), lhsT=pt[e][:, r * 128:(r + 1) * 128], rhs=vv[:, kb, :],
                                            start=(first and r % 2 == 0), stop=last, skip_group_check=True)
                if kind == "c":
                    for r in range(4):
                        inst = nc.tensor.matmul(P[UB][:, r * 128:(r + 1) * 128], lhsT=pt[e][:, r * 128:(r + 1) * 128],
                                                rhs=ovl[:, kb, :], start=(first and r == 0), stop=last, skip_group_check=True)
                return inst
            wr = [b_P[banks[0]], b_P[banks[1]]] + ([b_P[UB]] if kind == "c" else [])
            S.op("pe", mm, reads=[b_pt[e], bv, b_c], writes=wr)

        def emit_branch_out(kind, qb):
            banks = ACC_S if kind == "s" else ACC_X
            gcol = {"c": 0, "s": 1, "w": 2}[kind]
            o_ = osb[qb % 2]
            bo = b_osb[qb % 2]
            bb = [b_P[banks[0]], b_P[banks[1]]]

            def f1():
                inst = None
                for r in range(4):
                    den = acc_ap(banks, r)[:, 128:129]
                    inst = nc.vector.tensor_scalar(out=cf[:, r:r + 1], in0=den, scalar1=1e-30, scalar2=None, op0=ALU.max)
                return inst
            S.op("dve", f1, reads=bb, writes=[b_cf])
            S.op("dve", lambda: nc.vector.reciprocal(out=cf[:], in_=cf[:]), reads=[b_cf], writes=[b_cf])
            if kind == "c":
                S.op("dve", lambda: nc.vector.tensor_scalar(out=imp[:], in0=P[UB][:, 0:128], scalar1=cf[:, 0:1], scalar2=None,
                                                            op0=ALU.mult), reads=[b_P[UB], b_cf], writes=[b_sel])
                for r in range(1, 4):
                    S.op("dve", lambda: nc.vector.scalar_tensor_tensor(out=imp[:], in0=P[UB][:, r * 128:(r + 1) * 128],
                                                                       scalar=cf[:, r:r + 1], in1=imp[:],
                                                                       op0=ALU.mult, op1=ALU.add),
                         reads=[b_P[UB], b_cf, b_sel], writes=[b_sel])

            def f2():
                inst = None
                for r in range(4):
                    inst = nc.vector.tensor_tensor(out=cf[:, r:r + 1], in0=cf[:, r:r + 1],
                                                   in1=gts[:, qb, r * 3 + gcol:r * 3 + gcol + 1], op=ALU.mult)
                return inst
            S.op("dve", f2, reads=[b_cf, b_c], writes=[b_cf])

            def f3():
                inst = None
                for r in range(4):
                    a = acc_ap(banks, r)[:, 0:128]
                    if kind == "c":
                        inst = nc.vector.tensor_scalar(out=o_[:, r, :], in0=a, scalar1=cf[:, r:r + 1], scalar2=None, op0=ALU.mult)
                    else:
                        inst = nc.vector.scalar_tensor_tensor(out=o_[:, r, :], in0=a, scalar=cf[:, r:r + 1], in1=o_[:, r, :],
                                                              op0=ALU.mult, op1=ALU.add)
                return inst
            S.op("dve", f3, reads=bb + [b_cf], writes=[bo])

        def emit_select(qb):
            lo = 126 - 2 * qb
            S.op("dve", lambda: nc.vector.tensor_tensor(out=sc[:], in0=imp[:], in1=vm[:, lo:lo + 128], op=ALU.mult),
                 reads=[b_sel, b_c], writes=[b_sel])
            S.op("dve", lambda: nc.vector.tensor_tensor(out=sc[:], in0=sc[:], in1=am[:, lo:lo + 128], op=ALU.add),
                 reads=[b_sel, b_c], writes=[b_sel])
            S.op("dve", lambda: nc.vector.memset(sc[:, 0:1], 1e6), reads=[b_sel], writes=[b_sel])
            S.op("dve", lambda: nc.vector.max(out=m8a[:], in_=sc[:]), reads=[b_sel], writes=[b_sel])
            S.op("dve", lambda: nc.vector.match_replace(out=sc2[:], in_to_replace=m8a[:], in_values=sc[:], imm_value=-2.0),
                 reads=[b_sel], writes=[b_sel])
            S.op("dve", lambda: nc.vector.max(out=m8b[:], in_=sc2[:]), reads=[b_sel], writes=[b_sel])
            S.op("dve", lambda: nc.vector.tensor_scalar(out=ngm[:], in0=sc[:], scalar1=m8b[:, 7:8], scalar2=1.0,
                                                        op0=ALU.is_ge, op1=ALU.subtract), reads=[b_sel], writes=[b_sel])
            S.op("pe", lambda: nc.tensor.transpose(out=P[TRB][:, 0:128], in_=ngm[:], identity=identf[:]),
                 reads=[b_sel, b_c], writes=[b_P[TRB]])
            S.op("act", lambda: nc.scalar.copy(out=negT[qb % 2][:], in_=P[TRB][:, 0:128]),
                 reads=[b_P[TRB]], writes=[b_neg[qb % 2]])

        def emit_out(qb):
            o_ = osb[qb % 2]

            def tr():
                inst = None
                for r in range(4):
                    inst = nc.tensor.transpose(out=P[TRB][:, r * 128:(r + 1) * 128], in_=o_[:, r, :], identity=identf[:])
                return inst
            S.op("pe", tr, reads=[b_osb[qb % 2], b_c], writes=[b_P[TRB]])
            S.op("act", lambda: nc.scalar.copy(out=ost[qb % 2][:], in_=P[TRB][:].rearrange("p (r q) -> p r q", r=4)),
                 reads=[b_P[TRB]], writes=[b_ost[qb % 2]])
            out_toks.append(S.dma("sp", oT[:, :, qb * 128:(qb + 1) * 128].rearrange("r d q -> d r q"), ost[qb % 2][:],
                                  reads=[b_ost[qb % 2]]))

        stream = []
        def stage_A(qb):
            if qb % 4 == 0:
                stream.append(("f", lambda: load_q(qb // 4)))
            ncb = qb // 16 + 1
            for cb in range(ncb):
                stream.append(("u", ["c", qb, cb, cb == 0, cb == ncb - 1]))
            stream.append(("f", lambda: (emit_branch_out("c", qb), emit_select(qb))))

        def stage_B(qb):
            for kb in range(qb + 1):
                stream.append(("u", ["s", qb, kb, kb == 0, kb == qb]))
            stream.append(("f", lambda: emit_branch_out("s", qb)))
            k0 = max(0, qb - 4)
            for kb in range(k0, qb + 1):
                stream.append(("u", ["w", qb, kb, kb == k0, kb == qb]))
            stream.append(("f", lambda: (emit_branch_out("w", qb), emit_out(qb))))

        stage_A(0)
        for qb in range(NB):
            if qb + 1 < NB:
                stage_A(qb + 1)
            stage_B(qb)
        units = [it[1] for it in stream if it[0] == "u"]
        for i, u in enumerate(units):
            u.append(i)
        idx = 0
        seq = []
        for it in stream:
            seq.append(it)
        first_u = True
        upos = 0
        pending_st = None
        i = 0
        n = len(seq)
        def next_unit_index(j):
            while j < n and seq[j][0] != "u":
                j += 1
            return j
        j0 = next_unit_index(0)
        for it in seq[:j0]:
            it[1]()
        emit_st(seq[j0][1])
        j = j0
        while j < n:
            u = seq[j][1]
            jn = next_unit_index(j + 1)
            fitems = seq[j + 1:jn]
            if jn < n and not fitems:
                emit_st(seq[jn][1])
                emit_exp_pv(u)
            else:
                emit_exp_pv(u)
                for it in fitems:
                    it[1]()
                if jn < n:
                    emit_st(seq[jn][1])
            j = jn
        S.finish(out_toks)
        print("nsa program: ops=%d waits=%d" % (S.ninst, S.nwaits))
    return nc


def build_mixout(NT):
    nc = bass.Bass("TRN2", target_bir_lowering=False)
    xT = nc.dram_tensor("xT", [D_MODEL, NT], F32, kind="ExternalInput").ap()
    oT = nc.dram_tensor("oT", [16, 128, NT], F32, kind="ExternalInput").ap()
    gn = nc.dram_tensor("gn", [128, KC], F32, kind="ExternalInput").ap()
    w_out = nc.dram_tensor("w_out", [D_MODEL, D_MODEL], F32, kind="ExternalInput").ap()
    yT = nc.dram_tensor("yT", [D_MODEL, NT], F32, kind="ExternalOutput").ap()
    xTv = xT.rearrange("(c p) t -> p c t", p=128)
    yTv = yT.rearrange("(c p) t -> p c t", p=128)
    oTv = oT.rearrange("c p t -> p c t")
    wv = w_out.rearrange("(c p) f -> p c f", p=128)
    ntiles = NT // TT
    with contextlib.ExitStack() as st:
        S = Sched(nc, st)
        sb = lambda name, shape, dt: st.enter_context(nc.sbuf_tensor(name, shape, dt))
        P = [st.enter_context(nc.psum_tensor("ps%d" % i, [128, 512], F32)) for i in range(8)]
        b_P = [Buf("P%d" % i) for i in range(8)]
        xt = sb("xt", [128, KC, TT], F32)
        ot_ = sb("ot_", [128, KC, TT], F32)
        sq = sb("sq", [128, KC, TT], BF16)
        yn = sb("yn", [128, KC, TT], BF16)
        wo = sb("wo", [128, KC, D_MODEL], BF16)
        rs = [sb("rs%d" % i, [128, TT], F32) for i in range(2)]
        res = [sb("res%d" % i, [128, 2, TT], F32) for i in range(2)]
        g_sb = sb("g_sb", [128, KC], F32)
        ones = sb("ones", [128, 128], BF16)
        b_xt, b_ot, b_sq, b_yn, b_wo, b_c = (Buf(n) for n in "xt ot sq yn wo c".split())
        b_rs = [Buf("rs0"), Buf("rs1")]
        b_res = [Buf("r0"), Buf("r1")]
        S.dma("sp", g_sb[:], gn, writes=[b_c])
        S.op("dve", lambda: nc.vector.memset(ones[:], 1.0), writes=[b_c])
        for c4 in range(4):
            S.dma("pool", wo[:, c4 * 4:(c4 + 1) * 4, :], wv[:, c4 * 4:(c4 + 1) * 4, :], writes=[b_wo])
        out_toks = []
        pi = 0
        for t in range(ntiles):
            tsl = slice(t * TT, (t + 1) * TT)
            S.dma("sp", xt[:], xTv[:, :, tsl], writes=[b_xt])
            S.dma("sp", ot_[:], oTv[:, :, tsl], writes=[b_ot])
            S.op("act", lambda: nc.scalar.activation(out=sq[:], in_=ot_[:], func=AF.Square), reads=[b_ot], writes=[b_sq])
            for half in range(2):
                pp = P[pi % 8]; bp = b_P[pi % 8]; pi += 1

                def mm():
                    inst = None
                    for c in range(8):
                        inst = nc.tensor.matmul(pp[:], lhsT=ones[:], rhs=sq[:, half * 8 + c, :], start=(c == 0), stop=(c == 7))
                    return inst
                S.op("pe", mm, reads=[b_sq, b_c], writes=[bp])
                r_ = rs[half]; br = b_rs[half]
                S.op("dve", lambda: nc.vector.tensor_scalar(out=r_[:], in0=pp[:], scalar1=1.0 / 1024, scalar2=EPS,
                                                            op0=ALU.mult, op1=ALU.add), reads=[bp], writes=[br])
                S.op("dve", lambda: nc.vector.reciprocal(out=r_[:], in_=r_[:]), reads=[br], writes=[br])
                S.op("act", lambda: nc.scalar.activation(out=r_[:], in_=r_[:], func=AF.Sqrt), reads=[br], writes=[br])

            def mk():
                inst = None
                for c in range(KC):
                    inst = nc.vector.scalar_tensor_tensor(out=yn[:, c, :], in0=ot_[:, c, :], scalar=g_sb[:, c:c + 1],
                                                          in1=rs[c // 8][:], op0=ALU.mult, op1=ALU.mult)
                return inst
            S.op("dve", mk, reads=[b_ot, b_c, b_rs[0], b_rs[1]], writes=[b_yn])
            for dc in range(KC):
                pp = P[pi % 8]; bp = b_P[pi % 8]; pi += 1

                def mm():
                    inst = None
                    for c in range(KC):
                        inst = nc.tensor.matmul(pp[:], lhsT=wo[:, c, dc * 128:(dc + 1) * 128], rhs=yn[:, c, :],
                                                start=(c == 0), stop=(c == KC - 1))
                    return inst
                S.op("pe", mm, reads=[b_wo, b_yn], writes=[bp])
                ri = (dc // 2) % 2
                S.op("dve", lambda: nc.vector.tensor_tensor(out=res[ri][:, dc % 2, :], in0=pp[:], in1=xt[:, dc, :], op=ALU.add),
                     reads=[bp, b_xt], writes=[b_res[ri]])
                if dc % 2 == 1:
                    out_toks.append(S.dma("sp", yTv[:, dc - 1:dc + 1, tsl], res[ri][:], reads=[b_res[ri]]))
        S.finish(out_toks)
        print("mixout program: ops=%d waits=%d" % (S.ninst, S.nwaits))
    return nc


_PROGS = {}


def _prog(name, fn, *a):
    key = (name,) + a
    if key not in _PROGS:
        _PROGS[key] = fn(*a)
    return _PROGS[key]


def _run(nc, in_maps):
    res = run_bass_kernel_spmd(nc, in_maps, core_ids=list(range(NCORES)))
    return res.results


def _chunk_vec(v):
    return np.ascontiguousarray(np.asarray(v, np.float32).reshape(KC, 128).T)


def _tokmaj(v, nb):
    return np.ascontiguousarray(v.reshape(nb, 128, 128).transpose(1, 0, 2))


def kernel(x, ffn1_norm, ffn1_w1, ffn1_w3, ffn1_w2, mix_norm, w_in, gate_bias,
           q_norm_a, k_norm_a, q_norm_nsa, k_norm_nsa, cmp_pos, cmp_k_w1, cmp_k_w2,
           cmp_v_w1, cmp_v_w2, out_norm, w_out, ffn2_norm, ffn2_w1, ffn2_w3, ffn2_w2, rel_bias,
           n_layers=DEPTH):
    f32 = lambda a: np.ascontiguousarray(np.asarray(a, dtype=np.float32))
    x = f32(x)
    B, S_, D = x.shape
    NT = S_ // 2
    NB = S_ // 128
    rel_bias = f32(rel_bias)
    xs = [np.ascontiguousarray(x[c // 2, (c % 2) * NT:(c % 2 + 1) * NT].T) for c in range(NCORES)]
    p_ffn = _prog("ffn", build_ffn, NT)
    p_in = _prog("mixin", build_mixin, NT)
    p_dil = _prog("dil", build_dilated, S_, 4)
    p_nsa = _prog("nsa", build_nsa, S_)
    p_out = _prog("mixout", build_mixout, NT)
    ohd, exd, _ = toeplitz_consts("dil")
    ohw, exw, _ = toeplitz_consts("win")
    ohs, exs, _ = toeplitz_consts("sel")
    ident, J = host_consts()
    ovl, mcmp, rw, vm, am = nsa_consts(S_)

    def ffn(xs, g, w1, w3, w2):
        gn = _chunk_vec(g)
        w1, w3, w2 = f32(w1), f32(w3), f32(w2)
        r = _run(p_ffn, [{"xT": xs[c], "gn": gn, "w1": w1, "w3": w3, "w2": w2} for c in range(NCORES)])
        return [r[c]["yT"] for c in range(NCORES)]

    for l in range(n_layers):
        xs = ffn(xs, ffn1_norm[l], ffn1_w1[l], ffn1_w3[l], ffn1_w2[l])
        qkg = np.ascontiguousarray(np.stack([f32(q_norm_a[l]), f32(k_norm_a[l]), f32(q_norm_nsa[l]), f32(k_norm_nsa[l])], axis=1))
        gn = _chunk_vec(mix_norm[l])
        wi = f32(w_in[l])
        gb = f32(gate_bias[l]).reshape(1, 24)
        r = _run(p_in, [{"xT": xs[c], "gn": gn, "w_in": wi, "qkg": qkg, "gbias": gb} for c in range(NCORES)])
        dil_maps, nsa_maps = [], []
        posT = np.ascontiguousarray(f32(cmp_pos[l]).T)
        kg = f32(k_norm_nsa[l]).reshape(128, 1)
        kw1, kw2, vw1, vw2 = f32(cmp_k_w1[l]), f32(cmp_k_w2[l]), f32(cmp_v_w1[l]), f32(cmp_v_w2[l])
        for b in range(B):
            fm = np.concatenate([r[2 * b]["fmT"], r[2 * b + 1]["fmT"]], axis=2)
            vt = np.concatenate([r[2 * b]["vtok"], r[2 * b + 1]["vtok"]], axis=0)
            gt = np.concatenate([r[2 * b]["gates"], r[2 * b + 1]["gates"]], axis=0)
            for g in range(2):
                vh = np.stack([_tokmaj(vt[:, (4 * g + h) * 128:(4 * g + h + 1) * 128], NB) for h in range(4)], axis=0)
                dil_maps.append({"qT": np.ascontiguousarray(fm[4 * g:4 * g + 4]),
                                 "kT": np.ascontiguousarray(fm[8 + 4 * g:12 + 4 * g]),
                                 "v": np.ascontiguousarray(vh),
                                 "relb": np.ascontiguousarray(rel_bias[:, 4 * g:4 * g + 4]),
                                 "ohd": ohd, "exd": exd, "ident": ident, "J": J})
                nsa_maps.append({"qT": np.ascontiguousarray(fm[16 + 4 * g:20 + 4 * g]),
                                 "ksT": np.ascontiguousarray(fm[24 + g]), "kwT": np.ascontiguousarray(fm[26 + g]),
                                 "kcr": np.ascontiguousarray(fm[28 + g]), "vcr": np.ascontiguousarray(fm[30 + g]),
                                 "vs": _tokmaj(vt[:, 1024 + g * 128:1024 + (g + 1) * 128], NB),
                                 "vw": _tokmaj(vt[:, 1280 + g * 128:1280 + (g + 1) * 128], NB),
                                 "gat": np.ascontiguousarray(gt[:, 12 * g:12 * g + 12].reshape(NB, 128, 12).transpose(1, 0, 2)),
                                 "posT": posT, "kw1": kw1, "kw2": kw2, "vw1": vw1, "vw2": vw2, "kg": kg,
                                 "relb": np.ascontiguousarray(rel_bias[:, 8 + 4 * g:12 + 4 * g]),
                                 "ohw": ohw, "exw": exw, "ohs": ohs, "exs": exs, "ident": ident, "J": J,
                                 "ovl": ovl, "mcmp": mcmp, "rw": rw, "vm": vm, "am": am})
        rd = _run(p_dil, dil_maps)
        rn = _run(p_nsa, nsa_maps)
        gn = _chunk_vec(out_norm[l])
        wo = f32(w_out[l])
        maps = []
        for c in range(NCORES):
            b, hf = c // 2, c % 2
            sl = slice(hf * NT, (hf + 1) * NT)
            o16 = np.concatenate([rd[2 * b]["oT"][:, :, sl], rd[2 * b + 1]["oT"][:, :, sl],
                                  rn[2 * b]["oT"][:, :, sl], rn[2 * b + 1]["oT"][:, :, sl]], axis=0)
            maps.append({"xT": xs[c], "oT": np.ascontiguousarray(o16), "gn": gn, "w_out": wo})
        r = _run(p_out, maps)
        xs = [r[c]["yT"] for c in range(NCORES)]
        xs = ffn(xs, ffn2_norm[l], ffn2_w1[l], ffn2_w3[l], ffn2_w2[l])
    out = np.empty((B, S_, D), np.float32)
    for c in range(NCORES):
        out[c // 2, (c % 2) * NT:(c % 2 + 1) * NT] = xs[c].T
    return out
```

```python
import contextlib
import numpy as np
import concourse.bass as bass
import concourse.mybir as mybir
from concourse.bass_utils import run_bass_kernel_spmd

F32 = mybir.dt.float32
BF16 = mybir.dt.bfloat16
ALU = mybir.AluOpType
AF = mybir.ActivationFunctionType

D_MODEL = 2048
BATCH = 4
SEQ = 8192
DEPTH = 4
D_FF = 5632
EPS = 1e-6
NCORES = 8
KC = D_MODEL // 128
FC = D_FF // 128


class Buf:
    __slots__ = ("name", "w", "r")

    def __init__(self, name):
        self.name = name
        self.w = None
        self.r = {}


class Eng:
    def __init__(self, name, e, sem):
        self.name = name
        self.e = e
        self.sem = sem
        self.cnt = 0
        self.known = {}


class Sched:
    def __init__(self, nc, stack, n_dma_sems=6):
        self.nc = nc
        self.engs = {}
        for name, e in (("pe", nc.tensor), ("act", nc.scalar), ("dve", nc.vector),
                        ("pool", nc.gpsimd), ("sp", nc.sync)):
            sem = stack.enter_context(nc.semaphore("s_" + name))
            self.engs[name] = Eng(name, e, sem)
        self.dma_sems = {}
        for q in ("sp", "pool", "act"):
            lst = []
            for i in range(n_dma_sems):
                sem = stack.enter_context(nc.semaphore("d_%s%d" % (q, i)))
                lst.append([sem, 0])
            self.dma_sems[q] = [lst, 0]
        self.nwaits = 0
        self.ninst = 0
        self.snaps = {}

    def _wait(self, eng, tok):
        sem, val = tok
        if eng.known.get(id(sem), 0) >= val:
            return
        eng.e.wait_ge(sem, val)
        eng.known[id(sem)] = val
        self.nwaits += 1
        snap = self.snaps.get((id(sem), val))
        if snap:
            k = eng.known
            for s_, v_ in snap.items():
                if k.get(s_, 0) < v_:
                    k[s_] = v_

    def _deps(self, eng, reads, writes, skip_self):
        need = {}

        def add(tok):
            if tok is None:
                return
            sem, val = tok
            if skip_self and sem is eng.sem:
                return
            cur = need.get(id(sem))
            if cur is None or cur[1] < val:
                need[id(sem)] = tok

        for b in reads:
            add(b.w)
        for b in writes:
            add(b.w)
            for t in b.r.values():
                add(t)
        for tok in need.values():
            self._wait(eng, tok)

    def _commit(self, tok, reads, writes):
        sem, val = tok
        for b in reads:
            b.r[id(sem)] = tok
        for b in writes:
            b.w = tok
            b.r = {}

    def op(self, engname, fn, reads=(), writes=()):
        eng = self.engs[engname]
        self._deps(eng, reads, writes, skip_self=(engname == "pe"))
        inst = fn()
        eng.cnt += 1
        inst.then_inc(eng.sem, 1)
        tok = (eng.sem, eng.cnt)
        self.snaps[(id(eng.sem), eng.cnt)] = dict(eng.known)
        self._commit(tok, reads, writes)
        self.ninst += 1
        return tok

    def dma(self, q, out, in_, reads=(), writes=(), **kw):
        eng = self.engs[q]
        self._deps(eng, reads, writes, skip_self=False)
        lst, idx = self.dma_sems[q]
        slot = lst[idx]
        self.dma_sems[q][1] = (idx + 1) % len(lst)
        if slot[1] > 0:
            self._wait(eng, (slot[0], slot[1]))
        slot[1] += 16
        eng.e.dma_start(out=out, in_=in_, **kw).then_inc(slot[0], 16)
        tok = (slot[0], slot[1])
        self.snaps[(id(slot[0]), slot[1])] = dict(eng.known)
        self._commit(tok, reads, writes)
        self.ninst += 1
        return tok

    def fresh_engine_sems(self, stack, tag):
        for name, eng in self.engs.items():
            eng.sem = stack.enter_context(self.nc.semaphore("s_%s_%s" % (name, tag)))
            eng.cnt = 0

    def barrier(self):
        toks = [(e.sem, e.cnt) for e in self.engs.values() if e.cnt > 0]
        for q in self.dma_sems:
            for slot in self.dma_sems[q][0]:
                if slot[1] > 0:
                    toks.append((slot[0], slot[1]))
        for e in self.engs.values():
            for t in toks:
                if t[0] is not e.sem:
                    self._wait(e, t)
        self.snaps = {}

    def finish(self, toks):
        eng = self.engs["sp"]
        for t in toks:
            self._wait(eng, t)


class Ctx:
    def __init__(self, nc, stack):
        self.nc = nc
        self.S = Sched(nc, stack)
        self.P = [stack.enter_context(nc.psum_tensor("ps%d" % i, [128, 512], F32)) for i in range(8)]
        self.b_P = [Buf("P%d" % i) for i in range(8)]
        self.n = 0

    def uniq(self, name):
        self.n += 1
        return "%s_u%d" % (name, self.n)


def _uq(C, name):
    return name if C is None else C.uniq(name)


TT = 512
FG = 256
DG = 256


def build_ffn(NT, C=None, aps=None):
    if C:
        nc = C.nc
        xT, gn, w1, w3, w2, yT = aps
    else:
        nc = bass.Bass("TRN2", target_bir_lowering=False)
        xT = nc.dram_tensor("xT", [D_MODEL, NT], F32, kind="ExternalInput").ap()
        gn = nc.dram_tensor("gn", [128, KC], F32, kind="ExternalInput").ap()
        w1 = nc.dram_tensor("w1", [D_MODEL, D_FF], F32, kind="ExternalInput").ap()
        w3 = nc.dram_tensor("w3", [D_MODEL, D_FF], F32, kind="ExternalInput").ap()
        w2 = nc.dram_tensor("w2", [D_FF, D_MODEL], F32, kind="ExternalInput").ap()
        yT = nc.dram_tensor("yT", [D_MODEL, NT], F32, kind="ExternalOutput").ap()
    xTv = xT.rearrange("(c p) t -> p c t", p=128)
    yTv = yT.rearrange("(c p) t -> p c t", p=128)
    w1v = w1.rearrange("(c p) f -> p c f", p=128)
    w3v = w3.rearrange("(c p) f -> p c f", p=128)
    w2v = w2.rearrange("(c p) f -> p c f", p=128)
    ntiles = NT // TT
    with contextlib.ExitStack() as st:
        S = C.S if C else Sched(nc, st)
        sb = lambda name, shape, dt: st.enter_context(nc.sbuf_tensor(_uq(C, name), shape, dt))
        xt = sb("xt", [128, KC, TT], F32)
        xn = sb("xn", [128, KC, TT], BF16)
        ga = sb("ga", [128, FC, TT], BF16)
        wa = [sb("wa%d" % i, [128, KC, FG], BF16) for i in range(2)]
        wb = [sb("wb%d" % i, [128, KC, FG], BF16) for i in range(2)]
        wc = [sb("wc%d" % i, [128, FC, DG], BF16) for i in range(2)]
        ot = [sb("ot%d" % i, [128, DG // 128, TT], F32) for i in range(2)]
        sl = [sb("sl%d" % i, [128, TT], F32) for i in range(2)]
        rstd = sb("rstd", [128, TT], F32)
        g_sb = sb("g_sb", [128, KC], F32)
        ones = sb("ones", [128, 128], BF16)
        P = C.P if C else [st.enter_context(nc.psum_tensor("ps%d" % i, [128, 512], F32)) for i in range(8)]
        b_xt, b_xn, b_ga, b_rstd, b_g, b_ones = (Buf(n) for n in ("xt", "xn", "ga", "rstd", "g", "ones"))
        b_wa = [Buf("wa0"), Buf("wa1")]
        b_wc = [Buf("wc0"), Buf("wc1")]
        b_ot = [Buf("ot0"), Buf("ot1")]
        b_sl = [Buf("sl0"), Buf("sl1")]
        b_P = C.b_P if C else [Buf("P%d" % i) for i in range(8)]

        S.dma("sp", g_sb[:], gn, writes=[b_g])
        S.op("dve", lambda: nc.vector.memset(ones[:], 1.0), writes=[b_ones])

        nfg = D_FF // FG
        ndg = D_MODEL // DG
        slabs = []
        for t in range(ntiles):
            for j in range(nfg):
                slabs.append(("a", t, j))
            for j in range(ndg):
                slabs.append(("c", t, j))
        cnt = {"a": 0, "c": 0}
        slab_buf = []
        for kind, t, j in slabs:
            slab_buf.append(cnt[kind] % 2)
            cnt[kind] += 1

        w1b = nc.dram_tensor(_uq(C, "w1b"), [D_MODEL, D_FF], BF16, kind="Internal").ap()
        w3b = nc.dram_tensor(_uq(C, "w3b"), [D_MODEL, D_FF], BF16, kind="Internal").ap()
        w2b = nc.dram_tensor(_uq(C, "w2b"), [D_FF, D_MODEL], BF16, kind="Internal").ap()
        w1bv = w1b.rearrange("(c p) f -> p c f", p=128)
        w3bv = w3b.rearrange("(c p) f -> p c f", p=128)
        w2bv = w2b.rearrange("(c p) f -> p c f", p=128)
        b_c1 = [Buf("c1") for _ in range(nfg)]
        b_c2 = [Buf("c2") for _ in range(ndg)]
        for j in range(nfg):
            cs = slice(j * FG, (j + 1) * FG)
            S.dma("pool", w1b[:, cs], w1[:, cs], writes=[b_c1[j]])
            S.dma("pool", w3b[:, cs], w3[:, cs], writes=[b_c1[j]])
        for j in range(ndg):
            cs = slice(j * DG, (j + 1) * DG)
            S.dma("pool", w2b[:, cs], w2[:, cs], writes=[b_c2[j]])

        def load_slab(i):
            kind, t, j = slabs[i]
            bi = slab_buf[i]
            if kind == "a":
                S.dma("sp", wa[bi][:], w1bv[:, :, j * FG:(j + 1) * FG], reads=[b_c1[j]], writes=[b_wa[bi]])
                S.dma("sp", wb[bi][:], w3bv[:, :, j * FG:(j + 1) * FG], reads=[b_c1[j]], writes=[b_wa[bi]])
            else:
                S.dma("sp", wc[bi][:], w2bv[:, :, j * DG:(j + 1) * DG], reads=[b_c2[j]], writes=[b_wc[bi]])

        out_toks = []
        load_slab(0)
        si = 0
        pi = 0
        for t in range(ntiles):
            tsl = slice(t * TT, (t + 1) * TT)
            S.dma("sp", xt[:], xTv[:, :, tsl], writes=[b_xt])
            sq = ga[:, 0:KC, :]
            S.op("act", lambda: nc.scalar.activation(out=sq, in_=xt[:], func=AF.Square),
                 reads=[b_xt], writes=[b_ga])
            pss = P[pi % 8]; bp = b_P[pi % 8]; pi += 1

            def mm_ss():
                for c in range(KC):
                    i = nc.tensor.matmul(pss[:], lhsT=ones[:], rhs=ga[:, c, :], start=(c == 0), stop=(c == KC - 1))
                return i
            S.op("pe", mm_ss, reads=[b_ga, b_ones], writes=[bp])
            S.op("dve", lambda: nc.vector.tensor_scalar(out=rstd[:], in0=pss[:], scalar1=1.0 / D_MODEL, scalar2=EPS,
                                                        op0=ALU.mult, op1=ALU.add), reads=[bp], writes=[b_rstd])
            S.op("dve", lambda: nc.vector.reciprocal(out=rstd[:], in_=rstd[:]), reads=[b_rstd], writes=[b_rstd])
            S.op("act", lambda: nc.scalar.activation(out=rstd[:], in_=rstd[:], func=AF.Sqrt),
                 reads=[b_rstd], writes=[b_rstd])

            def mk_xn():
                for c in range(KC):
                    i = nc.vector.scalar_tensor_tensor(out=xn[:, c, :], in0=xt[:, c, :], scalar=g_sb[:, c:c + 1],
                                                       in1=rstd[:], op0=ALU.mult, op1=ALU.mult)
                return i
            S.op("dve", mk_xn, reads=[b_xt, b_rstd, b_g], writes=[b_xn])
            for j in range(nfg):
                bi = slab_buf[si]
                if si + 1 < len(slabs):
                    load_slab(si + 1)
                si += 1
                for q in range(FG // 128):
                    fc = j * (FG // 128) + q
                    p1 = P[pi % 8]; bp1 = b_P[pi % 8]; pi += 1
                    p3 = P[pi % 8]; bp3 = b_P[pi % 8]; pi += 1

                    def mm_up(p1=p1, p3=p3, q=q, bi=bi):
                        for c in range(KC):
                            nc.tensor.matmul(p1[:], lhsT=wa[bi][:, c, q * 128:(q + 1) * 128], rhs=xn[:, c, :],
                                             start=(c == 0), stop=(c == KC - 1))
                        for c in range(KC):
                            i = nc.tensor.matmul(p3[:], lhsT=wb[bi][:, c, q * 128:(q + 1) * 128], rhs=xn[:, c, :],
                                                 start=(c == 0), stop=(c == KC - 1))
                        return i
                    S.op("pe", mm_up, reads=[b_wa[bi], b_xn], writes=[bp1, bp3])
                    s_ = sl[fc % 2]; bs = b_sl[fc % 2]
                    S.op("act", lambda: nc.scalar.activation(out=s_[:], in_=p1[:], func=AF.Silu),
                         reads=[bp1], writes=[bs])
                    S.op("dve", lambda: nc.vector.tensor_tensor(out=ga[:, fc, :], in0=s_[:], in1=p3[:], op=ALU.mult),
                         reads=[bs, bp3], writes=[b_ga])
            for j in range(ndg):
                bi = slab_buf[si]
                if si + 1 < len(slabs):
                    load_slab(si + 1)
                si += 1
                o_ = ot[j % 2]; bo = b_ot[j % 2]
                for q in range(DG // 128):
                    dc = j * (DG // 128) + q
                    po = P[pi % 8]; bpo = b_P[pi % 8]; pi += 1

                    def mm_dn(po=po, q=q, bi=bi):
                        for f in range(FC):
                            i = nc.tensor.matmul(po[:], lhsT=wc[bi][:, f, q * 128:(q + 1) * 128], rhs=ga[:, f, :],
                                                 start=(f == 0), stop=(f == FC - 1))
                        return i
                    S.op("pe", mm_dn, reads=[b_wc[bi], b_ga], writes=[bpo])
                    S.op("dve", lambda: nc.vector.scalar_tensor_tensor(out=o_[:, q, :], in0=po[:], scalar=0.5,
                                                                       in1=xt[:, dc, :], op0=ALU.mult, op1=ALU.add),
                         reads=[bpo, b_xt], writes=[bo])
                dsl = slice(j * (DG // 128), (j + 1) * (DG // 128))
                out_toks.append(S.dma("pool", yTv[:, dsl, tsl], o_[:], reads=[bo]))
        if C:
            S.barrier()
        else:
            S.finish(out_toks)
        if not C:
            print("ffn program: ops=%d waits=%d" % (S.ninst, S.nwaits))
    return nc


IN_COLS = 5656
HD = 128
FM_CHUNKS = []
for i in range(8):
    FM_CHUNKS.append((0 + 128 * i, i, 0, True))
for i in range(8):
    FM_CHUNKS.append((1024 + 128 * i, 8 + i, 1, False))
for i in range(8):
    FM_CHUNKS.append((3072 + 128 * i, 16 + i, 2, True))
for i in range(2):
    FM_CHUNKS.append((4608 + 128 * i, 24 + i, 3, False))
for i in range(2):
    FM_CHUNKS.append((5120 + 128 * i, 26 + i, 3, False))
for i in range(2):
    FM_CHUNKS.append((4096 + 128 * i, 28 + i, -1, False))
for i in range(2):
    FM_CHUNKS.append((4352 + 128 * i, 30 + i, -1, False))
TM_GROUPS = [(2048, 256, 0), (2304, 256, 256), (2560, 256, 512), (2816, 256, 768),
             (4864, 256, 1024), (5376, 256, 1280)]


def build_mixin(NT, C=None, aps=None):
    if C:
        nc = C.nc
        xT, gn, w_in, qkg, gbias, fmT, vtok, gates = aps
    else:
        nc = bass.Bass("TRN2", target_bir_lowering=False)
        xT = nc.dram_tensor("xT", [D_MODEL, NT], F32, kind="ExternalInput").ap()
        gn = nc.dram_tensor("gn", [128, KC], F32, kind="ExternalInput").ap()
        w_in = nc.dram_tensor("w_in", [D_MODEL, IN_COLS], F32, kind="ExternalInput").ap()
        qkg = nc.dram_tensor("qkg", [128, 4], F32, kind="ExternalInput").ap()
        gbias = nc.dram_tensor("gbias", [1, 24], F32, kind="ExternalInput").ap()
        fmT = nc.dram_tensor("fmT", [32, 128, NT], BF16, kind="ExternalOutput").ap()
        vtok = nc.dram_tensor("vtok", [NT, 1536], BF16, kind="ExternalOutput").ap()
        gates = nc.dram_tensor("gates", [NT, 24], F32, kind="ExternalOutput").ap()
    xTv = xT.rearrange("(c p) t -> p c t", p=128)
    wv = w_in.rearrange("(c p) f -> p c f", p=128)
    ntiles = NT // TT
    with contextlib.ExitStack() as st:
        S = C.S if C else Sched(nc, st)
        sb = lambda name, shape, dt: st.enter_context(nc.sbuf_tensor(_uq(C, name), shape, dt))
        xt = sb("xt", [128, KC, TT], F32)
        xn = sb("xn", [128, KC, TT], BF16)
        sq = sb("sq", [128, KC, TT], BF16)
        ws = [sb("ws%d" % i, [128, KC, 256], BF16) for i in range(2)]
        wg = sb("wg", [128, KC, 24], BF16)
        rstd = sb("rstd", [128, TT], F32)
        hsq = [sb("hsq%d" % i, [128, TT], BF16) for i in range(2)]
        hr = [sb("hr%d" % i, [128, TT], F32) for i in range(2)]
        fo = [sb("fo%d" % i, [128, TT], BF16) for i in range(2)]
        to = [sb("to%d" % i, [128, 256], BF16) for i in range(2)]
        go = [sb("go%d" % i, [128, 24], F32) for i in range(2)]
        g_sb = sb("g_sb", [128, KC], F32)
        qkg_sb = sb("qkg_sb", [128, 4], F32)
        gb_sb = sb("gb_sb", [128, 24], F32)
        ones = sb("ones", [128, 128], BF16)
        P = C.P if C else [st.enter_context(nc.psum_tensor("ps%d" % i, [128, 512], F32)) for i in range(8)]
        b_xt, b_xn, b_sq, b_rstd, b_c = (Buf(n) for n in ("xt", "xn", "sq", "rstd", "consts"))
        b_ws = [Buf("ws0"), Buf("ws1")]
        b_wg = Buf("wg")
        b_hsq = [Buf("a"), Buf("b")]
        b_hr = [Buf("a"), Buf("b")]
        b_fo = [Buf("a"), Buf("b")]
        b_to = [Buf("a"), Buf("b")]
        b_go = [Buf("a"), Buf("b")]
        b_P = C.b_P if C else [Buf("P%d" % i) for i in range(8)]

        S.dma("sp", g_sb[:], gn, writes=[b_c])
        S.dma("sp", qkg_sb[:], qkg, writes=[b_c])
        S.dma("sp", gb_sb[:], gbias.partition_broadcast(128), writes=[b_c])
        S.op("dve", lambda: nc.vector.memset(ones[:], 1.0), writes=[b_c])
        S.dma("pool", wg[:], wv[:, :, 5632:5656], writes=[b_wg])

        fm_slabs = [(FM_CHUNKS[i], FM_CHUNKS[i + 1]) for i in range(0, len(FM_CHUNKS), 2)]
        per_tile = [("fm", s_) for s_ in fm_slabs] + [("tm", g_) for g_ in TM_GROUPS]
        slabs = []
        for t in range(ntiles):
            slabs += per_tile
        wib = nc.dram_tensor(_uq(C, "wib"), [D_MODEL, IN_COLS], BF16, kind="Internal").ap()
        wibv = wib.rearrange("(c p) f -> p c f", p=128)
        b_cw = {}
        for kind, info in per_tile:
            c0 = info[0][0] if kind == "fm" else info[0]
            b_cw[c0] = Buf("cw%d" % c0)
            S.dma("pool", wib[:, c0:c0 + 256], w_in[:, c0:c0 + 256], writes=[b_cw[c0]])

        def load_slab(i):
            kind, info = slabs[i]
            bi = i % 2
            c0 = info[0][0] if kind == "fm" else info[0]
            S.dma("sp", ws[bi][:], wibv[:, :, c0:c0 + 256], reads=[b_cw[c0]], writes=[b_ws[bi]])

        out_toks = []
        load_slab(0)
        si = 0
        pi = 0
        ei = 0
        for t in range(ntiles):
            tsl = slice(t * TT, (t + 1) * TT)
            S.dma("sp", xt[:], xTv[:, :, tsl], writes=[b_xt])
            S.op("act", lambda: nc.scalar.activation(out=sq[:], in_=xt[:], func=AF.Square),
                 reads=[b_xt], writes=[b_sq])
            pss = P[pi % 8]; bp = b_P[pi % 8]; pi += 1

            def mm_ss():
                for c in range(KC):
                    i = nc.tensor.matmul(pss[:], lhsT=ones[:], rhs=sq[:, c, :], start=(c == 0), stop=(c == KC - 1))
                return i
            S.op("pe", mm_ss, reads=[b_sq, b_c], writes=[bp])
            S.op("dve", lambda: nc.vector.tensor_scalar(out=rstd[:], in0=pss[:], scalar1=1.0 / D_MODEL, scalar2=EPS,
                                                        op0=ALU.mult, op1=ALU.add), reads=[bp], writes=[b_rstd])
            S.op("dve", lambda: nc.vector.reciprocal(out=rstd[:], in_=rstd[:]), reads=[b_rstd], writes=[b_rstd])
            S.op("act", lambda: nc.scalar.activation(out=rstd[:], in_=rstd[:], func=AF.Sqrt),
                 reads=[b_rstd], writes=[b_rstd])

            def mk_xn():
                for c in range(KC):
                    i = nc.vector.scalar_tensor_tensor(out=xn[:, c, :], in0=xt[:, c, :], scalar=g_sb[:, c:c + 1],
                                                       in1=rstd[:], op0=ALU.mult, op1=ALU.mult)
                return i
            S.op("dve", mk_xn, reads=[b_xt, b_rstd, b_c], writes=[b_xn])

            for kind, info in per_tile:
                bi = si % 2
                if si + 1 < len(slabs):
                    load_slab(si + 1)
                si += 1
                if kind == "fm":
                    for q, (c0, oc, gi, scaled) in enumerate(info):
                        pp = P[pi % 8]; bpp = b_P[pi % 8]; pi += 1

                        def mm(pp=pp, q=q, bi=bi):
                            for c in range(KC):
                                i = nc.tensor.matmul(pp[:], lhsT=ws[bi][:, c, q * 128:(q + 1) * 128], rhs=xn[:, c, :],
                                                     start=(c == 0), stop=(c == KC - 1))
                            return i
                        S.op("pe", mm, reads=[b_ws[bi], b_xn], writes=[bpp])
                        e = ei % 2; ei += 1
                        if gi < 0:
                            S.op("act", lambda: nc.scalar.copy(out=fo[e][:], in_=pp[:]), reads=[bpp], writes=[b_fo[e]])
                        else:
                            S.op("act", lambda: nc.scalar.activation(out=hsq[e][:], in_=pp[:], func=AF.Square),
                                 reads=[bpp], writes=[b_hsq[e]])
                            p2 = P[pi % 8]; bp2 = b_P[pi % 8]; pi += 1
                            S.op("pe", lambda: nc.tensor.matmul(p2[:], lhsT=ones[:], rhs=hsq[e][:], start=True, stop=True),
                                 reads=[b_hsq[e], b_c], writes=[bp2])
                            if scaled:
                                a1, a2 = 1.0, HD * EPS
                            else:
                                a1, a2 = 1.0 / HD, EPS
                            S.op("dve", lambda: nc.vector.tensor_scalar(out=hr[e][:], in0=p2[:], scalar1=a1, scalar2=a2,
                                                                        op0=ALU.mult, op1=ALU.add),
                                 reads=[bp2], writes=[b_hr[e]])
                            S.op("dve", lambda: nc.vector.reciprocal(out=hr[e][:], in_=hr[e][:]),
                                 reads=[b_hr[e]], writes=[b_hr[e]])
                            S.op("act", lambda: nc.scalar.activation(out=hr[e][:], in_=hr[e][:], func=AF.Sqrt),
                                 reads=[b_hr[e]], writes=[b_hr[e]])
                            S.op("dve", lambda: nc.vector.scalar_tensor_tensor(out=fo[e][:], in0=pp[:],
                                                                               scalar=qkg_sb[:, gi:gi + 1], in1=hr[e][:],
                                                                               op0=ALU.mult, op1=ALU.mult),
                                 reads=[bpp, b_hr[e], b_c], writes=[b_fo[e]])
                        out_toks.append(S.dma("pool", fmT[oc, :, tsl], fo[e][:], reads=[b_fo[e]]))
                else:
                    c0, wdt, vc0 = info
                    for sub in range(TT // 128):
                        pp = P[pi % 8]; bpp = b_P[pi % 8]; pi += 1

                        def mm(pp=pp, sub=sub, bi=bi):
                            for c in range(KC):
                                i = nc.tensor.matmul(pp[:, 0:256], lhsT=xn[:, c, sub * 128:(sub + 1) * 128],
                                                     rhs=ws[bi][:, c, :], start=(c == 0), stop=(c == KC - 1))
                            return i
                        S.op("pe", mm, reads=[b_ws[bi], b_xn], writes=[bpp])
                        e = ei % 2; ei += 1
                        S.op("act", lambda: nc.scalar.copy(out=to[e][:], in_=pp[:, 0:256]), reads=[bpp], writes=[b_to[e]])
                        r0 = t * TT + sub * 128
                        if C:
                            dst = vtok[vc0 // 128:vc0 // 128 + 2, :, r0 // 128, :].rearrange("h p d -> p h d")
                            out_toks.append(S.dma("pool", dst, to[e][:].rearrange("p (h d) -> p h d", h=2), reads=[b_to[e]]))
                        else:
                            out_toks.append(S.dma("pool", vtok[r0:r0 + 128, vc0:vc0 + 256], to[e][:], reads=[b_to[e]]))
            for sub in range(TT // 128):
                pp = P[pi % 8]; bpp = b_P[pi % 8]; pi += 1

                def mm(pp=pp, sub=sub):
                    for c in range(KC):
                        i = nc.tensor.matmul(pp[:, 0:24], lhsT=xn[:, c, sub * 128:(sub + 1) * 128],
                                             rhs=wg[:, c, :], start=(c == 0), stop=(c == KC - 1))
                    return i
                S.op("pe", mm, reads=[b_wg, b_xn], writes=[bpp])
                e = ei % 2; ei += 1
                S.op("dve", lambda: nc.vector.tensor_tensor(out=go[e][:], in0=pp[:, 0:24], in1=gb_sb[:], op=ALU.add),
                     reads=[bpp, b_c], writes=[b_go[e]])
                S.op("act", lambda: nc.scalar.activation(out=go[e][:], in_=go[e][:], func=AF.Sigmoid),
                     reads=[b_go[e]], writes=[b_go[e]])
                r0 = t * TT + sub * 128
                if C:
                    out_toks.append(S.dma("pool", gates[:, r0 // 128, :], go[e][:], reads=[b_go[e]]))
                else:
                    out_toks.append(S.dma("pool", gates[r0:r0 + 128, :], go[e][:], reads=[b_go[e]]))
        if C:
            S.barrier()
        else:
            S.finish(out_toks)
        if not C:
            print("mixin program: ops=%d waits=%d" % (S.ninst, S.nwaits))
    return nc

NEG = -30000.0
N_BUCKETS = 32


def rel_bucket_np(dist):
    n = np.maximum(dist, 0)
    nf = np.maximum(n, 1).astype(np.float32)
    lb = 16 + (np.log(nf / np.float32(16)) / np.float32(np.log(2048 / 16)) * np.float32(16)).astype(np.int32)
    return np.where(n < 16, n, np.minimum(lb, 31)).astype(np.int64)


def toeplitz_consts(kind):
    if kind == "dil":
        L, base = 3072, 511
    elif kind == "win":
        L, base = 1024, 127
    else:
        L, base = 2048, 127
    dist = np.arange(L) - base
    oh = np.zeros((32, L), np.float32)
    oh[rel_bucket_np(dist), np.arange(L)] = 1.0
    extra = np.zeros((1, L), np.float32)
    if kind == "dil":
        m = ((dist <= 128).astype(np.int64) + ((dist % 4 == 0) & (dist <= 512)) + ((dist % 16 == 0) & (dist <= 2048)))
        m = np.where(dist < 0, 0, m)
        extra[0] = np.where(m > 0, np.log(np.maximum(m, 1)), NEG)
    elif kind == "win":
        extra[0] = np.where((dist >= 0) & (dist < 512), 0.0, NEG)
    else:
        extra[0] = np.where(dist >= 0, 0.0, NEG)
        oh[31, :] -= 1.0
    return oh, extra, L


def gen_toeplitz(nc, S, st, tag, relb_ap, oh_ap, extra_ap, L, Lw, nh, W, b_W, J, b_c, P, b_P, C=None):
    sb = lambda name, shape, dt: st.enter_context(nc.sbuf_tensor(_uq(C, tag + name), shape, dt))
    fvd = nc.dram_tensor(_uq(C, tag + "fvd"), [nh, L], BF16, kind="Internal")
    relb = sb("relb", [32, nh], F32)
    oh = sb("oh", [32, L], F32)
    ex = sb("ex", [1, L], F32)
    one1 = sb("one1", [1, nh], F32)
    fv = sb("fv", [nh, L], BF16)
    hk = sb("hk", [128, Lw], BF16)
    b_in, b_fv, b_fvd, b_hk = Buf("in"), Buf("fv"), Buf("fvd"), Buf("hk")
    S.dma("sp", relb[:], relb_ap, writes=[b_in])
    S.dma("sp", oh[:], oh_ap, writes=[b_in])
    S.dma("sp", ex[:], extra_ap, writes=[b_in])
    S.op("dve", lambda: nc.vector.memset(one1[:], 1.0), writes=[b_in])
    for c in range(L // 512):
        cs = slice(c * 512, (c + 1) * 512)
        pp, bp = P[c % 2], b_P[c % 2]

        def mm():
            nc.tensor.matmul(pp[0:nh, :], lhsT=relb[:], rhs=oh[:, cs], start=True, stop=False)
            return nc.tensor.matmul(pp[0:nh, :], lhsT=one1[:], rhs=ex[:, cs], start=False, stop=True)
        S.op("pe", mm, reads=[b_in], writes=[bp])
        S.op("dve", lambda: nc.vector.tensor_copy(out=fv[:, cs], in_=pp[0:nh, :]), reads=[bp], writes=[b_fv])
    S.dma("sp", fvd.ap(), fv[:], reads=[b_fv], writes=[b_fvd])
    for h in range(nh):
        src = bass.AP(fvd, h * L, [[1, 128], [1, Lw]])
        S.dma("sp", hk[:], src, reads=[b_fvd], writes=[b_hk])
        c0 = 0
        ci = 0
        while c0 < Lw:
            n = min(512, Lw - c0)
            pp, bp = P[ci % 2], b_P[ci % 2]
            S.op("pe", lambda: nc.tensor.matmul(pp[:, 0:n], lhsT=J[:], rhs=hk[:, c0:c0 + n], start=True, stop=True),
                 reads=[b_hk, b_c], writes=[bp])
            S.op("dve", lambda: nc.vector.tensor_copy(out=W[:, h, c0:c0 + n], in_=pp[:, 0:n]), reads=[bp], writes=[b_W])
            c0 += n
            ci += 1


def host_consts():
    ident = np.eye(128, dtype=np.float32)
    J = np.ascontiguousarray(ident[::-1])
    return ident, J


def build_dilated(SQ=SEQ, NH=4, C=None, aps=None):
    NB = SQ // 128
    NG = SQ // 512
    LD = 3072
    LW = 2944
    if C:
        nc = C.nc
        qT, kT, v, relb, ohd, exd, identd, Jd, oT = aps
    else:
        nc = bass.Bass("TRN2", target_bir_lowering=False)
        qT = nc.dram_tensor("qT", [NH, 128, SQ], BF16, kind="ExternalInput").ap()
        kT = nc.dram_tensor("kT", [NH, 128, SQ], BF16, kind="ExternalInput").ap()
        v = nc.dram_tensor("v", [NH, 128, NB, 128], BF16, kind="ExternalInput").ap()
        relb = nc.dram_tensor("relb", [32, NH], F32, kind="ExternalInput").ap()
        ohd = nc.dram_tensor("ohd", [32, LD], F32, kind="ExternalInput").ap()
        exd = nc.dram_tensor("exd", [1, LD], F32, kind="ExternalInput").ap()
        identd = nc.dram_tensor("ident", [128, 128], F32, kind="ExternalInput").ap()
        Jd = nc.dram_tensor("J", [128, 128], F32, kind="ExternalInput").ap()
        oT = nc.dram_tensor("oT", [NH, 128, SQ], F32, kind="ExternalOutput").ap()
    with contextlib.ExitStack() as st:
        S = C.S if C else Sched(nc, st)
        sb = lambda name, shape, dt: st.enter_context(nc.sbuf_tensor(_uq(C, name), shape, dt))
        P = C.P if C else [st.enter_context(nc.psum_tensor("ps%d" % i, [128, 512], F32)) for i in range(8)]
        b_P = C.b_P if C else [Buf("P%d" % i) for i in range(8)]
        b_c = Buf("consts")
        identf = sb("identf", [128, 128], F32)
        identb = sb("identb", [128, 128], BF16)
        Jb = sb("Jb", [128, 128], BF16)
        S.dma("sp", identf[:], identd, writes=[b_c])
        S.dma("pool", identb[:], identd, writes=[b_c])
        S.dma("pool", Jb[:], Jd, writes=[b_c])
        W = sb("W", [128, NH, LW], BF16)
        b_W = Buf("W")
        with contextlib.ExitStack() as st2:
            gen_toeplitz(nc, S, st2, "d_", relb, ohd, exd, LD, LW, NH, W, b_W, Jb, b_c, P, b_P, C)
        qs = [sb("qs%d" % i, [128, SQ], BF16) for i in range(2)]
        ks = [sb("ks%d" % i, [128, SQ], BF16) for i in range(2)]
        vs = [sb("vs%d" % i, [128, NB, 129], BF16) for i in range(2)]
        b_hd = [Buf("hd0"), Buf("hd1")]
        for i in range(2):
            S.op("dve", lambda: nc.vector.memset(vs[i][:, :, 128:129], 1.0), writes=[b_hd[i]])
        NPT = 3
        pt = [sb("pt%d" % i, [128, 512], BF16) for i in range(NPT)]
        b_pt = [Buf("pt%d" % i) for i in range(NPT)]
        osb = [sb("osb%d" % i, [128, 4, 128], F32) for i in range(2)]
        b_osb = [Buf("osb0"), Buf("osb1")]
        rd = [sb("rd%d" % i, [128, 4], F32) for i in range(2)]
        b_rd = [Buf("rd0"), Buf("rd1")]
        ost = [sb("ost%d" % i, [128, 512], F32) for i in range(2)]
        b_ost = [Buf("ost0"), Buf("ost1")]
        ST_BANKS = [0, 1, 2]
        ACC = [(3, 4), (5, 6)]
        TRB = 7

        def load_head(h):
            i = h % 2
            S.dma("sp", qs[i][:], qT[h], writes=[b_hd[i]])
            S.dma("sp", ks[i][:], kT[h], writes=[b_hd[i]])
            S.dma("sp", vs[i][:, :, 0:128], v[h], writes=[b_hd[i]])

        units = []
        for h in range(NH):
            for G in range(NG):
                kbs = list(range(max(0, 4 * G - 16), 4 * G + 4))
                for kb in kbs:
                    units.append((h, G, kb, kb == kbs[0], kb == kbs[-1]))
        out_toks = []
        gcount = [0]

        def emit_st(u, ui):
            h, G, kb, first, last = u
            i = h % 2
            bank = ST_BANKS[ui % 3]
            d = 4 * G - kb

            def mm():
                nc.tensor.matmul(P[bank][:], lhsT=ks[i][:, kb * 128:(kb + 1) * 128], rhs=qs[i][:, G * 512:(G + 1) * 512],
                                 start=True, stop=False)
                return nc.tensor.matmul(P[bank][:], lhsT=identb[:], rhs=W[:, h, 128 * (d + 3):128 * (d + 3) + 512],
                                        start=False, stop=True)
            S.op("pe", mm, reads=[b_hd[i], b_W, b_c], writes=[b_P[bank]])

        def emit_exp_pv(u, ui):
            h, G, kb, first, last = u
            i = h % 2
            bank = ST_BANKS[ui % 3]
            e = ui % NPT
            S.op("act", lambda: nc.scalar.activation(out=pt[e][:], in_=P[bank][:], func=AF.Exp),
                 reads=[b_P[bank]], writes=[b_pt[e]])
            gi = (h * NG + G) % 2
            a0, a1 = ACC[gi]

            def mm():
                inst = None
                for qi in range(4):
                    db = 4 * G + qi - kb
                    if db < 0 or db > 16:
                        continue
                    kfirst = max(0, 4 * G + qi - 16)
                    bank_a = a0 if qi < 2 else a1
                    inst = nc.tensor.matmul(P[bank_a][:, (qi % 2) * 129:(qi % 2) * 129 + 129],
                                            lhsT=pt[e][:, qi * 128:(qi + 1) * 128], rhs=vs[i][:, kb, :],
                                            start=(kb == kfirst and qi % 2 == 0), stop=(db == 0), skip_group_check=True)
                return inst
            S.op("pe", mm, reads=[b_pt[e], b_hd[i]], writes=[b_P[a0], b_P[a1]])

        def emit_norm(u):
            h, G, kb, first, last = u
            gi = (h * NG + G) % 2
            a0, a1 = ACC[gi]
            o_, r_ = osb[gi], rd[gi]

            def f1():
                inst = None
                for qi in range(4):
                    bank_a = a0 if qi < 2 else a1
                    c = (qi % 2) * 129
                    inst = nc.vector.reciprocal(out=r_[:, qi:qi + 1], in_=P[bank_a][:, c + 128:c + 129])
                return inst
            S.op("dve", f1, reads=[b_P[a0], b_P[a1]], writes=[b_rd[gi]])

            def f2():
                inst = None
                for qi in range(4):
                    bank_a = a0 if qi < 2 else a1
                    c = (qi % 2) * 129
                    inst = nc.vector.tensor_scalar(out=o_[:, qi, :], in0=P[bank_a][:, c:c + 128], scalar1=r_[:, qi:qi + 1],
                                                   scalar2=None, op0=ALU.mult)
                return inst
            S.op("dve", f2, reads=[b_P[a0], b_P[a1], b_rd[gi]], writes=[b_osb[gi]])

        def emit_tr(u):
            h, G, kb, first, last = u
            gi = (h * NG + G) % 2
            o_ = osb[gi]

            def tr():
                inst = None
                for qi in range(4):
                    inst = nc.tensor.transpose(out=P[TRB][:, qi * 128:(qi + 1) * 128], in_=o_[:, qi, :], identity=identf[:])
                return inst
            S.op("pe", tr, reads=[b_osb[gi], b_c], writes=[b_P[TRB]])
            S.op("act", lambda: nc.scalar.copy(out=ost[gi][:], in_=P[TRB][:]), reads=[b_P[TRB]], writes=[b_ost[gi]])
            out_toks.append(S.dma("sp", oT[h, :, G * 512:(G + 1) * 512], ost[gi][:], reads=[b_ost[gi]]))

        pending = []
        load_head(0)
        emit_st(units[0], 0)
        for ui, u in enumerate(units):
            if ui + 1 < len(units):
                emit_st(units[ui + 1], ui + 1)
            emit_exp_pv(u, ui)
            if u[1] == 0 and u[3] and u[0] + 1 < NH:
                load_head(u[0] + 1)
            if u[4]:
                emit_norm(u)
                pending.append([2, u])
            np_ = []
            for p_ in pending:
                p_[0] -= 1
                if p_[0] <= 0:
                    emit_tr(p_[1])
                else:
                    np_.append(p_)
            pending = np_
        for p_ in pending:
            emit_tr(p_[1])
        if C:
            S.barrier()
        else:
            S.finish(out_toks)
        if not C:
            print("dilated program: ops=%d waits=%d" % (S.ninst, S.nwaits))
    return nc


def nsa_consts(SQ=SEQ):
    ncmp = SQ // 16
    ovl = np.zeros((ncmp, 128), np.float32)
    c = np.arange(ncmp)[:, None] * 16
    n = np.arange(128)[None, :] * 64
    ovl[:] = ((c < n + 64) & (c + 32 > n)).astype(np.float32)
    ovl[ncmp - 1] = 0.0
    ovl = np.ascontiguousarray(ovl.reshape(ncmp // 128, 128, 128).transpose(1, 0, 2))
    j = np.arange(128)[:, None, None]
    o = np.arange(16)[None, :, None]
    i = np.arange(128)[None, None, :]
    mcmp = np.where(16 * j + 31 <= 128 * o + i, 0.0, NEG).astype(np.float32)
    rw = np.zeros((128, SQ), np.float32)
    rw[np.arange(SQ) // 64, np.arange(SQ)] = 1.0
    ii = np.arange(128)[:, None]
    rel = np.arange(256)[None, :] - 126
    ci = (ii >= 64).astype(np.int64)
    valid = rel <= ci
    forced = (rel == ci) | (rel == ci - 1)
    vm = (valid & ~forced).astype(np.float32)
    am = np.where(forced, 1e6, np.where(valid, 0.0, -1.0)).astype(np.float32)
    return ovl, mcmp, rw, vm, am


def build_nsa(SQ=SEQ, C=None, aps=None, gcol0=0):
    NB = SQ // 128
    NCH = SQ // 512
    LWN, LWW = 1024, 640
    LSN, LSW = 2048, 1792
    GW = 24 if C else 12
    if C:
        nc = C.nc
        (qT, ksT, kwT, vsd, vwd, kcr, vcr, gat, posT, kw1, kw2, vw1, vw2, kg, relb, ohw, exw, ohs, exs,
         identd, Jd, ovld, mcmpd, rwd, vmd, amd, oT) = aps
    else:
        nc = bass.Bass("TRN2", target_bir_lowering=False)
        dt_in = lambda name, shape, dt: nc.dram_tensor(name, shape, dt, kind="ExternalInput").ap()
        qT = dt_in("qT", [4, 128, SQ], BF16)
        ksT = dt_in("ksT", [128, SQ], BF16)
        kwT = dt_in("kwT", [128, SQ], BF16)
        vsd = dt_in("vs", [128, NB, 128], BF16)
        vwd = dt_in("vw", [128, NB, 128], BF16)
        kcr = dt_in("kcr", [128, SQ], BF16)
        vcr = dt_in("vcr", [128, SQ], BF16)
        gat = dt_in("gat", [128, NB, 12], F32)
        posT = dt_in("posT", [128, 32], F32)
        kw1 = dt_in("kw1", [4096, 512], F32)
        kw2 = dt_in("kw2", [512, 128], F32)
        vw1 = dt_in("vw1", [4096, 512], F32)
        vw2 = dt_in("vw2", [512, 128], F32)
        kg = dt_in("kg", [128, 1], F32)
        relb = dt_in("relb", [32, 4], F32)
        ohw = dt_in("ohw", [32, LWN], F32)
        exw = dt_in("exw", [1, LWN], F32)
        ohs = dt_in("ohs", [32, LSN], F32)
        exs = dt_in("exs", [1, LSN], F32)
        identd = dt_in("ident", [128, 128], F32)
        Jd = dt_in("J", [128, 128], F32)
        ovld = dt_in("ovl", [128, SQ // 2048, 128], F32)
        mcmpd = dt_in("mcmp", [128, 16, 128], F32)
        rwd = dt_in("rw", [128, SQ], F32)
        vmd = dt_in("vm", [128, 256], F32)
        amd = dt_in("am", [128, 256], F32)
        oT = nc.dram_tensor("oT", [4, 128, SQ], F32, kind="ExternalOutput").ap()
    NCMP = SQ // 16
    NCB = NCMP // 128
    with contextlib.ExitStack() as st:
        S = C.S if C else Sched(nc, st)
        sb = lambda name, shape, dt: st.enter_context(nc.sbuf_tensor(_uq(C, name), shape, dt))
        P = C.P if C else [st.enter_context(nc.psum_tensor("ps%d" % i, [128, 512], F32)) for i in range(8)]
        b_P = C.b_P if C else [Buf("P%d" % i) for i in range(8)]
        b_c = Buf("consts")
        identf = sb("identf", [128, 128], F32)
        identb = sb("identb", [128, 128], BF16)
        Jb = sb("Jb", [128, 128], BF16)
        ones = sb("ones", [128, 128], BF16)
        ovl = sb("ovl_sb", [128, SQ // 2048, 128], BF16)
        mcmp = sb("mcmp_sb", [128, 16, 128], BF16)
        rw = sb("rw_sb", [128, SQ], BF16)
        vm = sb("vm_sb", [128, 256], F32)
        am = sb("am_sb", [128, 256], F32)
        gts = sb("gts", [128, NB, GW], F32)
        kg_sb = sb("kg_sb", [128, 1], F32)
        S.dma("sp", identf[:], identd, writes=[b_c])
        S.dma("pool", identb[:], identd, writes=[b_c])
        S.dma("pool", Jb[:], Jd, writes=[b_c])
        S.dma("pool", ovl[:], ovld, writes=[b_c])
        S.dma("pool", mcmp[:], mcmpd, writes=[b_c])
        S.dma("pool", rw[:], rwd[:, 0:SQ], writes=[b_c])
        S.dma("sp", vm[:], vmd, writes=[b_c])
        S.dma("sp", am[:], amd, writes=[b_c])
        S.dma("sp", gts[:], gat, writes=[b_c])
        S.dma("sp", kg_sb[:], kg, writes=[b_c])
        b31 = sb("b31", [128, 4], F32)
        S.dma("sp", b31[:], relb[31:32, :].partition_broadcast(128), writes=[b_c])
        S.op("dve", lambda: nc.vector.memset(ones[:], 1.0), writes=[b_c])
        Ww = sb("Ww", [128, 4, LWW], BF16)
        Ws = sb("Ws", [128, 4, LSW], BF16)
        b_W = Buf("W")
        with contextlib.ExitStack() as st2:
            gen_toeplitz(nc, S, st2, "w_", relb, ohw, exw, LWN, LWW, 4, Ww, b_W, Jb, b_c, P, b_P, C)
        S.barrier()
        with contextlib.ExitStack() as st2:
            gen_toeplitz(nc, S, st2, "s_", relb, ohs, exs, LSN, LSW, 4, Ws, b_W, Jb, b_c, P, b_P, C)
        S.barrier()
        kcT = sb("kcT", [128, NCMP], BF16)
        vca = sb("vca", [128, NCB, 129], BF16)
        b_kc = Buf("kc")
        S.op("dve", lambda: nc.vector.memset(vca[:, :, 128:129], 1.0), writes=[b_kc])
        with contextlib.ExitStack() as st2:
            sb2 = lambda name, shape, dt: st2.enter_context(nc.sbuf_tensor(_uq(C, name), shape, dt))
            raw = sb2("raw", [128, SQ], BF16)
            D = sb2("D", [128, 32, NCMP], BF16)
            W1 = sb2("W1", [128, 32, 512], BF16)
            W2 = sb2("W2", [128, 4, 128], BF16)
            pos_sb = sb2("pos_sb", [128, 32], F32)
            hx = sb2("hx", [128, NCMP], F32)
            hu = sb2("hu", [128, NCMP], F32)
            hid = sb2("hid", [128, 4, NCMP], BF16)
            ksq = sb2("ksq", [128, NCMP], BF16)
            krs = sb2("krs", [128, NCMP], F32)
            b_raw, b_D, b_W1, b_W2, b_pos, b_h, b_hid, b_k = (Buf(n) for n in "raw D W1 W2 pos h hid k".split())
            S.dma("sp", pos_sb[:], posT, writes=[b_pos])
            for which in range(2):
                rawd, w1d, w2d = (kcr, kw1, kw2) if which == 0 else (vcr, vw1, vw2)
                S.dma("sp", raw[:], rawd, writes=[b_raw])
                S.dma("pool", W1[:], w1d.rearrange("(j d) h -> d j h", d=128), writes=[b_W1])
                S.dma("pool", W2[:], w2d.rearrange("(c p) d -> p c d", p=128), writes=[b_W2])
                S.op("dve", lambda: nc.vector.memset(D[:], 0.0), writes=[b_D])

                def mkD():
                    inst = None
                    for j in range(32):
                        inst = nc.vector.tensor_scalar(out=D[:, j, 0:NCMP - 1], in0=raw[:, j:j + 16 * (NCMP - 2) + 1:16],
                                                       scalar1=pos_sb[:, j:j + 1], scalar2=None, op0=ALU.add)
                    return inst
                S.op("dve", mkD, reads=[b_raw, b_pos], writes=[b_D])
                for hc in range(4):
                    pp, bp = P[hc % 2], b_P[hc % 2]

                    def mm():
                        inst = None
                        for j in range(32):
                            inst = nc.tensor.matmul(pp[:, 0:NCMP], lhsT=W1[:, j, hc * 128:(hc + 1) * 128], rhs=D[:, j, :],
                                                    start=(j == 0), stop=(j == 31))
                        return inst
                    S.op("pe", mm, reads=[b_W1, b_D], writes=[bp])
                    S.op("act", lambda: nc.scalar.activation(out=hu[:], in_=pp[:, 0:NCMP], func=AF.Square), reads=[bp], writes=[b_h])
                    S.op("dve", lambda: nc.vector.tensor_scalar(out=hu[:], in0=hu[:], scalar1=0.044715, scalar2=1.0,
                                                                op0=ALU.mult, op1=ALU.add), reads=[b_h], writes=[b_h])
                    S.op("dve", lambda: nc.vector.tensor_tensor(out=hu[:], in0=hu[:], in1=pp[:, 0:NCMP], op=ALU.mult),
                         reads=[b_h, bp], writes=[b_h])
                    S.op("act", lambda: nc.scalar.activation(out=hu[:], in_=hu[:], func=AF.Sigmoid, scale=1.5957691216),
                         reads=[b_h], writes=[b_h])
                    S.op("dve", lambda: nc.vector.tensor_tensor(out=hid[:, hc, :], in0=hu[:], in1=pp[:, 0:NCMP], op=ALU.mult),
                         reads=[b_h, bp], writes=[b_hid])
                if which == 0:
                    pp, bp = P[2], b_P[2]

                    def mm():
                        inst = None
                        for hc in range(4):
                            inst = nc.tensor.matmul(pp[:, 0:NCMP], lhsT=W2[:, hc, :], rhs=hid[:, hc, :], start=(hc == 0), stop=(hc == 3))
                        return inst
                    S.op("pe", mm, reads=[b_W2, b_hid], writes=[bp])
                    S.op("act", lambda: nc.scalar.activation(out=ksq[:], in_=pp[:, 0:NCMP], func=AF.Square), reads=[bp], writes=[b_k])
                    p2, bp2 = P[3], b_P[3]
                    S.op("pe", lambda: nc.tensor.matmul(p2[:, 0:NCMP], lhsT=ones[:], rhs=ksq[:], start=True, stop=True),
                         reads=[b_k, b_c], writes=[bp2])
                    S.op("dve", lambda: nc.vector.tensor_scalar(out=krs[:], in0=p2[:, 0:NCMP], scalar1=1.0 / HD, scalar2=EPS,
                                                                op0=ALU.mult, op1=ALU.add), reads=[bp2], writes=[b_k])
                    S.op("dve", lambda: nc.vector.reciprocal(out=krs[:], in_=krs[:]), reads=[b_k], writes=[b_k])
                    S.op("act", lambda: nc.scalar.activation(out=krs[:], in_=krs[:], func=AF.Sqrt), reads=[b_k], writes=[b_k])
                    S.op("dve", lambda: nc.vector.scalar_tensor_tensor(out=kcT[:], in0=pp[:, 0:NCMP], scalar=kg_sb[:, 0:1], in1=krs[:],
                                                                       op0=ALU.mult, op1=ALU.mult),
                         reads=[bp, b_k, b_c], writes=[b_kc])
                else:
                    for cb in range(NCB):
                        pp, bp = P[2 + cb % 2], b_P[2 + cb % 2]

                        def mm():
                            inst = None
                            for hc in range(4):
                                inst = nc.tensor.matmul(pp[:, 0:128], lhsT=hid[:, hc, cb * 128:(cb + 1) * 128], rhs=W2[:, hc, :],
                                                        start=(hc == 0), stop=(hc == 3))
                            return inst
                        S.op("pe", mm, reads=[b_W2, b_hid], writes=[bp])
                        S.op("dve", lambda: nc.vector.tensor_copy(out=vca[:, cb, 0:128], in_=pp[:, 0:128]),
                             reads=[bp], writes=[b_kc])
        S.barrier()
        ks_sb = sb("ks_sb", [128, SQ], BF16)
        kw_sb = sb("kw_sb", [128, SQ], BF16)
        vsa = sb("vsa", [128, NB, 129], BF16)
        vwa = sb("vwa", [128, NB, 129], BF16)
        b_kv = Buf("kv")
        S.dma("sp", ks_sb[:], ksT, writes=[b_kv])
        S.dma("sp", kw_sb[:], kwT, writes=[b_kv])
        S.dma("sp", vsa[:, :, 0:128], vsd, writes=[b_kv])
        S.dma("sp", vwa[:, :, 0:128], vwd, writes=[b_kv])
        S.op("dve", lambda: nc.vector.memset(vsa[:, :, 128:129], 1.0), writes=[b_kv])
        S.op("dve", lambda: nc.vector.memset(vwa[:, :, 128:129], 1.0), writes=[b_kv])
        qb_ = [sb("qb%d" % i, [128, 4, 512], BF16) for i in range(2)]
        b_q = [Buf("q0"), Buf("q1")]
        NPT = 3
        pt = [sb("pt%d" % i, [128, 512], BF16) for i in range(NPT)]
        b_pt = [Buf("pt%d" % i) for i in range(NPT)]
        osb = [sb("osb%d" % i, [128, 4, 128], F32) for i in range(2)]
        b_osb = [Buf("osb0"), Buf("osb1")]
        negT = [sb("negT%d" % i, [128, 4, 128], BF16) for i in range(2)]
        b_neg = [Buf("n0"), Buf("n1")]
        cf = sb("cf", [128, 4], F32)
        b_cf = Buf("cf")
        imp = sb("imp", [128, 128], F32)
        sc = sb("sc", [128, 128], F32)
        sc2 = sb("sc2", [128, 128], F32)
        m8a = sb("m8a", [128, 8], F32)
        m8b = sb("m8b", [128, 8], F32)
        ngm = sb("ngm", [128, 128], F32)
        b_sel = Buf("sel")
        ost = [sb("ost%d" % i, [128, 4, 128], F32) for i in range(2)]
        b_ost = [Buf("ost0"), Buf("ost1")]
        STB = [0, 1]
        ACC_X = (2, 3)
        ACC_S = (4, 5)
        UB = 6
        TRB = 7
        out_toks = []
        uctr = [0]

        def q_tile(qb):
            return qb_[(qb // 4) % 2][:, :, (qb % 4) * 128:(qb % 4) * 128 + 128]

        def load_q(ch):
            S.dma("sp", qb_[ch % 2][:], qT[:, :, ch * 512:(ch + 1) * 512].rearrange("r d q -> d r q"), writes=[b_q[ch % 2]])

        def emit_st(u):
            kind, qb, kb, first, last = u[:5]
            ui = u[5]
            bank = STB[ui % 2]
            bq = b_q[(qb // 4) % 2]
            qt = q_tile(qb)
            P3 = P[bank][:].rearrange("p (r q) -> p r q", r=4)
            if kind == "c":
                diag = (kb == qb // 16)

                def mm():
                    inst = nc.tensor.matmul(P3, lhsT=kcT[:, kb * 128:(kb + 1) * 128], rhs=qt, start=True, stop=True)
                    if diag:
                        for r in range(4):
                            inst = nc.tensor.matmul(P[bank][:, r * 128:(r + 1) * 128], lhsT=identb[:], rhs=mcmp[:, qb % 16, :],
                                                    start=False, stop=(r == 3), skip_group_check=True)
                    return inst
                S.op("pe", mm, reads=[b_kc, bq, b_c], writes=[b_P[bank]])
            elif kind == "s":
                d = min(qb - kb, 13)
                nb_ = negT[qb % 2]

                def mm():
                    nc.tensor.matmul(P3, lhsT=ks_sb[:, kb * 128:(kb + 1) * 128], rhs=qt, start=True, stop=True)
                    if qb - kb < 13:
                        nc.tensor.matmul(P3, lhsT=identb[:], rhs=Ws[:, :, d * 128:(d + 1) * 128], start=False, stop=False,
                                         skip_group_check=True)
                    return nc.tensor.matmul(P3, lhsT=rw[:, kb * 128:(kb + 1) * 128], rhs=nb_[:],
                                            start=False, stop=True, skip_group_check=True)
                S.op("pe", mm, reads=[b_kv, bq, b_c, b_W, b_neg[qb % 2]], writes=[b_P[bank]])
            else:
                d = qb - kb

                def mm():
                    nc.tensor.matmul(P3, lhsT=kw_sb[:, kb * 128:(kb + 1) * 128], rhs=qt, start=True, stop=True)
                    return nc.tensor.matmul(P3, lhsT=identb[:], rhs=Ww[:, :, d * 128:(d + 1) * 128], start=False, stop=True,
                                            skip_group_check=True)
                S.op("pe", mm, reads=[b_kv, bq, b_c, b_W], writes=[b_P[bank]])

        def acc_ap(banks, r):
            return P[banks[r // 2]][:, (r % 2) * 129:(r % 2) * 129 + 129]

        def emit_exp_pv(u):
            kind, qb, kb, first, last = u[:5]
            ui = u[5]
            bank = STB[ui % 2]
            e = ui % NPT
            S.op("act", lambda: nc.scalar.activation(out=pt[e][:], in_=P[bank][:], func=AF.Exp),
                 reads=[b_P[bank]], writes=[b_pt[e]])
            if kind == "c":
                banks, vv, bv = ACC_X, vca, b_kc
            elif kind == "s":
                banks, vv, bv = ACC_S, vsa, b_kv
            else:
                banks, vv, bv = ACC_X, vwa, b_kv

            def mm():
                inst = None
                for r in range(4):
                    inst = nc.tensor.matmul(acc_ap(banks, r), lhsT=pt[e][:, r * 128:(r + 1) * 128], rhs=vv[:, kb, :],
                                            start=(first and r % 2 == 0), stop=last, skip_group_check=True)
                if kind == "c":
                    for r in range(4):
                        inst = nc.tensor.matmul(P[UB][:, r * 128:(r + 1) * 128], lhsT=pt[e][:, r * 128:(r + 1) * 128],
                                                rhs=ovl[:, kb, :], start=(first and r == 0), stop=last, skip_group_check=True)
                return inst
            wr = [b_P[banks[0]], b_P[banks[1]]] + ([b_P[UB]] if kind == "c" else [])
            S.op("pe", mm, reads=[b_pt[e], bv, b_c], writes=wr)

        def emit_branch_out(kind, qb):
            banks = ACC_S if kind == "s" else ACC_X
            gcol = {"c": 0, "s": 1, "w": 2}[kind]
            o_ = osb[qb % 2]
            bo = b_osb[qb % 2]
            bb = [b_P[banks[0]], b_P[banks[1]]]

            def f1():
                inst = None
                for r in range(4):
                    den = acc_ap(banks, r)[:, 128:129]
                    inst = nc.vector.tensor_scalar(out=cf[:, r:r + 1], in0=den, scalar1=1e-30, scalar2=None, op0=ALU.max)
                return inst
            S.op("dve", f1, reads=bb, writes=[b_cf])
            S.op("dve", lambda: nc.vector.reciprocal(out=cf[:], in_=cf[:]), reads=[b_cf], writes=[b_cf])
            if kind == "c":
                S.op("dve", lambda: nc.vector.tensor_scalar(out=imp[:], in0=P[UB][:, 0:128], scalar1=cf[:, 0:1], scalar2=None,
                                                            op0=ALU.mult), reads=[b_P[UB], b_cf], writes=[b_sel])
                for r in range(1, 4):
                    S.op("dve", lambda: nc.vector.scalar_tensor_tensor(out=imp[:], in0=P[UB][:, r * 128:(r + 1) * 128],
                                                                       scalar=cf[:, r:r + 1], in1=imp[:],
                                                                       op0=ALU.mult, op1=ALU.add),
                         reads=[b_P[UB], b_cf, b_sel], writes=[b_sel])

            def f2():
                inst = None
                for r in range(4):
                    inst = nc.vector.tensor_tensor(out=cf[:, r:r + 1], in0=cf[:, r:r + 1],
                                                   in1=gts[:, qb, gcol0 + r * 3 + gcol:gcol0 + r * 3 + gcol + 1], op=ALU.mult)
                return inst
            S.op("dve", f2, reads=[b_cf, b_c], writes=[b_cf])

            def f3():
                inst = None
                for r in range(4):
                    a = acc_ap(banks, r)[:, 0:128]
                    if kind == "c":
                        inst = nc.vector.tensor_scalar(out=o_[:, r, :], in0=a, scalar1=cf[:, r:r + 1], scalar2=None, op0=ALU.mult)
                    else:
                        inst = nc.vector.scalar_tensor_tensor(out=o_[:, r, :], in0=a, scalar=cf[:, r:r + 1], in1=o_[:, r, :],
                                                              op0=ALU.mult, op1=ALU.add)
                return inst
            S.op("dve", f3, reads=bb + [b_cf], writes=[bo])

        def emit_select(qb):
            lo = 126 - 2 * qb
            S.op("dve", lambda: nc.vector.tensor_tensor(out=sc[:], in0=imp[:], in1=vm[:, lo:lo + 128], op=ALU.mult),
                 reads=[b_sel, b_c], writes=[b_sel])
            S.op("dve", lambda: nc.vector.tensor_tensor(out=sc[:], in0=sc[:], in1=am[:, lo:lo + 128], op=ALU.add),
                 reads=[b_sel, b_c], writes=[b_sel])
            S.op("dve", lambda: nc.vector.memset(sc[:, 0:1], 1e6), reads=[b_sel], writes=[b_sel])
            S.op("dve", lambda: nc.vector.max(out=m8a[:], in_=sc[:]), reads=[b_sel], writes=[b_sel])
            S.op("dve", lambda: nc.vector.match_replace(out=sc2[:], in_to_replace=m8a[:], in_values=sc[:], imm_value=-2.0),
                 reads=[b_sel], writes=[b_sel])
            S.op("dve", lambda: nc.vector.max(out=m8b[:], in_=sc2[:]), reads=[b_sel], writes=[b_sel])
            S.op("dve", lambda: nc.vector.tensor_scalar(out=ngm[:], in0=sc[:], scalar1=m8b[:, 7:8], scalar2=1.0,
                                                        op0=ALU.is_ge, op1=ALU.subtract), reads=[b_sel], writes=[b_sel])
            S.op("pe", lambda: nc.tensor.transpose(out=P[TRB][:, 0:128], in_=ngm[:], identity=identf[:]),
                 reads=[b_sel, b_c], writes=[b_P[TRB]])
            def mkneg():
                inst = None
                for r in range(4):
                    inst = nc.scalar.activation(out=negT[qb % 2][:, r, :], in_=P[TRB][:, 0:128], func=AF.Identity,
                                                bias=b31[:, r:r + 1], scale=-NEG)
                return inst
            S.op("act", mkneg, reads=[b_P[TRB], b_c], writes=[b_neg[qb % 2]])

        def emit_out(qb):
            o_ = osb[qb % 2]

            def tr():
                inst = None
                for r in range(4):
                    inst = nc.tensor.transpose(out=P[TRB][:, r * 128:(r + 1) * 128], in_=o_[:, r, :], identity=identf[:])
                return inst
            S.op("pe", tr, reads=[b_osb[qb % 2], b_c], writes=[b_P[TRB]])
            S.op("act", lambda: nc.scalar.copy(out=ost[qb % 2][:], in_=P[TRB][:].rearrange("p (r q) -> p r q", r=4)),
                 reads=[b_P[TRB]], writes=[b_ost[qb % 2]])
            out_toks.append(S.dma("sp", oT[:, :, qb * 128:(qb + 1) * 128].rearrange("r d q -> d r q"), ost[qb % 2][:],
                                  reads=[b_ost[qb % 2]]))

        stream = []
        def stage_A(qb):
            if qb % 4 == 0:
                stream.append(("f", lambda: load_q(qb // 4)))
            ncb = qb // 16 + 1
            for cb in range(ncb):
                stream.append(("u", ["c", qb, cb, cb == 0, cb == ncb - 1]))
            stream.append(("f", lambda: (emit_branch_out("c", qb), emit_select(qb))))

        def stage_B(qb):
            for kb in range(qb + 1):
                stream.append(("u", ["s", qb, kb, kb == 0, kb == qb]))
            stream.append(("f", lambda: emit_branch_out("s", qb)))
            k0 = max(0, qb - 4)
            for kb in range(k0, qb + 1):
                stream.append(("u", ["w", qb, kb, kb == k0, kb == qb]))
            stream.append(("f", lambda: (emit_branch_out("w", qb), emit_out(qb))))

        stage_A(0)
        for qb in range(NB):
            if qb + 1 < NB:
                stage_A(qb + 1)
            stage_B(qb)
        units = [it[1] for it in stream if it[0] == "u"]
        for i, u in enumerate(units):
            u.append(i)
        idx = 0
        seq = []
        for it in stream:
            seq.append(it)
        first_u = True
        upos = 0
        pending_st = None
        i = 0
        n = len(seq)
        def next_unit_index(j):
            while j < n and seq[j][0] != "u":
                j += 1
            return j
        j0 = next_unit_index(0)
        for it in seq[:j0]:
            it[1]()
        emit_st(seq[j0][1])
        j = j0
        while j < n:
            u = seq[j][1]
            jn = next_unit_index(j + 1)
            fitems = seq[j + 1:jn]
            if jn < n and not fitems:
                emit_st(seq[jn][1])
                emit_exp_pv(u)
            else:
                emit_exp_pv(u)
                for it in fitems:
                    it[1]()
                if jn < n:
                    emit_st(seq[jn][1])
            j = jn
        if C:
            S.barrier()
        else:
            S.finish(out_toks)
        if not C:
            print("nsa program: ops=%d waits=%d" % (S.ninst, S.nwaits))
    return nc


def build_mixout(NT, C=None, aps=None):
    if C:
        nc = C.nc
        xT, oT, gn, w_out, yT = aps
    else:
        nc = bass.Bass("TRN2", target_bir_lowering=False)
        xT = nc.dram_tensor("xT", [D_MODEL, NT], F32, kind="ExternalInput").ap()
        oT = nc.dram_tensor("oT", [16, 128, NT], F32, kind="ExternalInput").ap()
        gn = nc.dram_tensor("gn", [128, KC], F32, kind="ExternalInput").ap()
        w_out = nc.dram_tensor("w_out", [D_MODEL, D_MODEL], F32, kind="ExternalInput").ap()
        yT = nc.dram_tensor("yT", [D_MODEL, NT], F32, kind="ExternalOutput").ap()
    xTv = xT.rearrange("(c p) t -> p c t", p=128)
    yTv = yT.rearrange("(c p) t -> p c t", p=128)
    oTv = oT.rearrange("c p t -> p c t")
    wv = w_out.rearrange("(c p) f -> p c f", p=128)
    ntiles = NT // TT
    with contextlib.ExitStack() as st:
        S = C.S if C else Sched(nc, st)
        sb = lambda name, shape, dt: st.enter_context(nc.sbuf_tensor(_uq(C, name), shape, dt))
        P = C.P if C else [st.enter_context(nc.psum_tensor("ps%d" % i, [128, 512], F32)) for i in range(8)]
        b_P = C.b_P if C else [Buf("P%d" % i) for i in range(8)]
        xt = sb("xt", [128, KC, TT], F32)
        ot_ = sb("ot_", [128, KC, TT], F32)
        sq = sb("sq", [128, KC, TT], BF16)
        yn = sb("yn", [128, KC, TT], BF16)
        wo = sb("wo", [128, KC, D_MODEL], BF16)
        rs = [sb("rs%d" % i, [128, TT], F32) for i in range(2)]
        res = [sb("res%d" % i, [128, 2, TT], F32) for i in range(2)]
        g_sb = sb("g_sb", [128, KC], F32)
        ones = sb("ones", [128, 128], BF16)
        b_xt, b_ot, b_sq, b_yn, b_wo, b_c = (Buf(n) for n in "xt ot sq yn wo c".split())
        b_rs = [Buf("rs0"), Buf("rs1")]
        b_res = [Buf("r0"), Buf("r1")]
        S.dma("sp", g_sb[:], gn, writes=[b_c])
        S.op("dve", lambda: nc.vector.memset(ones[:], 1.0), writes=[b_c])
        for c4 in range(4):
            S.dma("pool", wo[:, c4 * 4:(c4 + 1) * 4, :], wv[:, c4 * 4:(c4 + 1) * 4, :], writes=[b_wo])
        out_toks = []
        pi = 0
        for t in range(ntiles):
            tsl = slice(t * TT, (t + 1) * TT)
            S.dma("sp", xt[:], xTv[:, :, tsl], writes=[b_xt])
            S.dma("sp", ot_[:], oTv[:, :, tsl], writes=[b_ot])
            S.op("act", lambda: nc.scalar.activation(out=sq[:], in_=ot_[:], func=AF.Square), reads=[b_ot], writes=[b_sq])
            for half in range(2):
                pp = P[pi % 8]; bp = b_P[pi % 8]; pi += 1

                def mm():
                    inst = None
                    for c in range(8):
                        inst = nc.tensor.matmul(pp[:], lhsT=ones[:], rhs=sq[:, half * 8 + c, :], start=(c == 0), stop=(c == 7))
                    return inst
                S.op("pe", mm, reads=[b_sq, b_c], writes=[bp])
                r_ = rs[half]; br = b_rs[half]
                S.op("dve", lambda: nc.vector.tensor_scalar(out=r_[:], in0=pp[:], scalar1=1.0 / 1024, scalar2=EPS,
                                                            op0=ALU.mult, op1=ALU.add), reads=[bp], writes=[br])
                S.op("dve", lambda: nc.vector.reciprocal(out=r_[:], in_=r_[:]), reads=[br], writes=[br])
                S.op("act", lambda: nc.scalar.activation(out=r_[:], in_=r_[:], func=AF.Sqrt), reads=[br], writes=[br])

            def mk():
                inst = None
                for c in range(KC):
                    inst = nc.vector.scalar_tensor_tensor(out=yn[:, c, :], in0=ot_[:, c, :], scalar=g_sb[:, c:c + 1],
                                                          in1=rs[c // 8][:], op0=ALU.mult, op1=ALU.mult)
                return inst
            S.op("dve", mk, reads=[b_ot, b_c, b_rs[0], b_rs[1]], writes=[b_yn])
            for dc in range(KC):
                pp = P[pi % 8]; bp = b_P[pi % 8]; pi += 1

                def mm():
                    inst = None
                    for c in range(KC):
                        inst = nc.tensor.matmul(pp[:], lhsT=wo[:, c, dc * 128:(dc + 1) * 128], rhs=yn[:, c, :],
                                                start=(c == 0), stop=(c == KC - 1))
                    return inst
                S.op("pe", mm, reads=[b_wo, b_yn], writes=[bp])
                ri = (dc // 2) % 2
                S.op("dve", lambda: nc.vector.tensor_tensor(out=res[ri][:, dc % 2, :], in0=pp[:], in1=xt[:, dc, :], op=ALU.add),
                     reads=[bp, b_xt], writes=[b_res[ri]])
                if dc % 2 == 1:
                    out_toks.append(S.dma("sp", yTv[:, dc - 1:dc + 1, tsl], res[ri][:], reads=[b_res[ri]]))
        if C:
            S.barrier()
        else:
            S.finish(out_toks)
        if not C:
            print("mixout program: ops=%d waits=%d" % (S.ninst, S.nwaits))
    return nc


_PROGS = {}


def _prog(name, fn, *a):
    key = (name,) + a
    if key not in _PROGS:
        _PROGS[key] = fn(*a)
    return _PROGS[key]


def _run(nc, in_maps):
    res = run_bass_kernel_spmd(nc, in_maps, core_ids=list(range(NCORES)))
    return res.results


def _chunk_vec(v):
    return np.ascontiguousarray(np.asarray(v, np.float32).reshape(KC, 128).T)


def _tokmaj(v, nb):
    return np.ascontiguousarray(v.reshape(nb, 128, 128).transpose(1, 0, 2))


def kernel_unfused(x, ffn1_norm, ffn1_w1, ffn1_w3, ffn1_w2, mix_norm, w_in, gate_bias,
           q_norm_a, k_norm_a, q_norm_nsa, k_norm_nsa, cmp_pos, cmp_k_w1, cmp_k_w2,
           cmp_v_w1, cmp_v_w2, out_norm, w_out, ffn2_norm, ffn2_w1, ffn2_w3, ffn2_w2, rel_bias,
           n_layers=DEPTH):
    f32 = lambda a: np.ascontiguousarray(np.asarray(a, dtype=np.float32))
    x = f32(x)
    B, S_, D = x.shape
    NT = S_ // 2
    NB = S_ // 128
    rel_bias = f32(rel_bias)
    xs = [np.ascontiguousarray(x[c // 2, (c % 2) * NT:(c % 2 + 1) * NT].T) for c in range(NCORES)]
    p_ffn = _prog("ffn", build_ffn, NT)
    p_in = _prog("mixin", build_mixin, NT)
    p_dil = _prog("dil", build_dilated, S_, 4)
    p_nsa = _prog("nsa", build_nsa, S_)
    p_out = _prog("mixout", build_mixout, NT)
    ohd, exd, _ = toeplitz_consts("dil")
    ohw, exw, _ = toeplitz_consts("win")
    ohs, exs, _ = toeplitz_consts("sel")
    ident, J = host_consts()
    ovl, mcmp, rw, vm, am = nsa_consts(S_)

    def ffn(xs, g, w1, w3, w2):
        gn = _chunk_vec(g)
        w1, w3, w2 = f32(w1), f32(w3), f32(w2)
        r = _run(p_ffn, [{"xT": xs[c], "gn": gn, "w1": w1, "w3": w3, "w2": w2} for c in range(NCORES)])
        return [r[c]["yT"] for c in range(NCORES)]

    for l in range(n_layers):
        xs = ffn(xs, ffn1_norm[l], ffn1_w1[l], ffn1_w3[l], ffn1_w2[l])
        qkg = np.ascontiguousarray(np.stack([f32(q_norm_a[l]), f32(k_norm_a[l]), f32(q_norm_nsa[l]), f32(k_norm_nsa[l])], axis=1))
        gn = _chunk_vec(mix_norm[l])
        wi = f32(w_in[l])
        gb = f32(gate_bias[l]).reshape(1, 24)
        r = _run(p_in, [{"xT": xs[c], "gn": gn, "w_in": wi, "qkg": qkg, "gbias": gb} for c in range(NCORES)])
        dil_maps, nsa_maps = [], []
        posT = np.ascontiguousarray(f32(cmp_pos[l]).T)
        kg = f32(k_norm_nsa[l]).reshape(128, 1)
        kw1, kw2, vw1, vw2 = f32(cmp_k_w1[l]), f32(cmp_k_w2[l]), f32(cmp_v_w1[l]), f32(cmp_v_w2[l])
        for b in range(B):
            fm = np.concatenate([r[2 * b]["fmT"], r[2 * b + 1]["fmT"]], axis=2)
            vt = np.concatenate([r[2 * b]["vtok"], r[2 * b + 1]["vtok"]], axis=0)
            gt = np.concatenate([r[2 * b]["gates"], r[2 * b + 1]["gates"]], axis=0)
            for g in range(2):
                vh = np.stack([_tokmaj(vt[:, (4 * g + h) * 128:(4 * g + h + 1) * 128], NB) for h in range(4)], axis=0)
                dil_maps.append({"qT": np.ascontiguousarray(fm[4 * g:4 * g + 4]),
                                 "kT": np.ascontiguousarray(fm[8 + 4 * g:12 + 4 * g]),
                                 "v": np.ascontiguousarray(vh),
                                 "relb": np.ascontiguousarray(rel_bias[:, 4 * g:4 * g + 4]),
                                 "ohd": ohd, "exd": exd, "ident": ident, "J": J})
                nsa_maps.append({"qT": np.ascontiguousarray(fm[16 + 4 * g:20 + 4 * g]),
                                 "ksT": np.ascontiguousarray(fm[24 + g]), "kwT": np.ascontiguousarray(fm[26 + g]),
                                 "kcr": np.ascontiguousarray(fm[28 + g]), "vcr": np.ascontiguousarray(fm[30 + g]),
                                 "vs": _tokmaj(vt[:, 1024 + g * 128:1024 + (g + 1) * 128], NB),
                                 "vw": _tokmaj(vt[:, 1280 + g * 128:1280 + (g + 1) * 128], NB),
                                 "gat": np.ascontiguousarray(gt[:, 12 * g:12 * g + 12].reshape(NB, 128, 12).transpose(1, 0, 2)),
                                 "posT": posT, "kw1": kw1, "kw2": kw2, "vw1": vw1, "vw2": vw2, "kg": kg,
                                 "relb": np.ascontiguousarray(rel_bias[:, 8 + 4 * g:12 + 4 * g]),
                                 "ohw": ohw, "exw": exw, "ohs": ohs, "exs": exs, "ident": ident, "J": J,
                                 "ovl": ovl, "mcmp": mcmp, "rw": rw, "vm": vm, "am": am})
        rd = _run(p_dil, dil_maps)
        rn = _run(p_nsa, nsa_maps)
        gn = _chunk_vec(out_norm[l])
        wo = f32(w_out[l])
        maps = []
        for c in range(NCORES):
            b, hf = c // 2, c % 2
            sl = slice(hf * NT, (hf + 1) * NT)
            o16 = np.concatenate([rd[2 * b]["oT"][:, :, sl], rd[2 * b + 1]["oT"][:, :, sl],
                                  rn[2 * b]["oT"][:, :, sl], rn[2 * b + 1]["oT"][:, :, sl]], axis=0)
            maps.append({"xT": xs[c], "oT": np.ascontiguousarray(o16), "gn": gn, "w_out": wo})
        r = _run(p_out, maps)
        xs = [r[c]["yT"] for c in range(NCORES)]
        xs = ffn(xs, ffn2_norm[l], ffn2_w1[l], ffn2_w3[l], ffn2_w2[l])
    out = np.empty((B, S_, D), np.float32)
    for c in range(NCORES):
        out[c // 2, (c % 2) * NT:(c % 2 + 1) * NT] = xs[c].T
    return out


def build_fused(n_layers=DEPTH, SQ=SEQ):
    nc = bass.Bass("TRN2", target_bir_lowering=False)
    L = DEPTH
    NB = SQ // 128
    din = lambda name, shape, dt=F32: nc.dram_tensor(name, shape, dt, kind="ExternalInput").ap()
    dsc = lambda name, shape, dt=F32: nc.dram_tensor(name, shape, dt, kind="Internal").ap()
    xT = din("xT", [D_MODEL, SQ])
    yT = nc.dram_tensor("yT", [D_MODEL, SQ], F32, kind="ExternalOutput").ap()
    W = {}
    for nm, shp in (("ffn1_w1", [L, D_MODEL, D_FF]), ("ffn1_w3", [L, D_MODEL, D_FF]), ("ffn1_w2", [L, D_FF, D_MODEL]),
                    ("ffn2_w1", [L, D_MODEL, D_FF]), ("ffn2_w3", [L, D_MODEL, D_FF]), ("ffn2_w2", [L, D_FF, D_MODEL]),
                    ("w_in", [L, D_MODEL, IN_COLS]), ("w_out", [L, D_MODEL, D_MODEL]),
                    ("cmp_k_w1", [L, 4096, 512]), ("cmp_k_w2", [L, 512, 128]),
                    ("cmp_v_w1", [L, 4096, 512]), ("cmp_v_w2", [L, 512, 128]),
                    ("rel_bias", [32, 16]),
                    ("gn_ffn1", [L, 128, KC]), ("gn_mix", [L, 128, KC]), ("gn_out", [L, 128, KC]), ("gn_ffn2", [L, 128, KC]),
                    ("qkg", [L, 128, 4]), ("gbias", [L, 1, 24]), ("posT", [L, 128, 32]), ("kg", [L, 128, 1]),
                    ("ohd", [32, 3072]), ("exd", [1, 3072]), ("ohw", [32, 1024]), ("exw", [1, 1024]),
                    ("ohs", [32, 2048]), ("exs", [1, 2048]), ("ident", [128, 128]), ("J", [128, 128]),
                    ("ovl", [128, SQ // 2048, 128]), ("mcmp", [128, 16, 128]), ("rw", [128, SQ]),
                    ("vm", [128, 256]), ("am", [128, 256])):
        W[nm] = din(nm, shp)
    s1 = dsc("s1", [D_MODEL, SQ])
    s2 = dsc("s2", [D_MODEL, SQ])
    s3 = dsc("s3", [D_MODEL, SQ])
    fmT = dsc("fmT", [32, 128, SQ], BF16)
    vL = dsc("vL", [12, 128, NB, 128], BF16)
    gL = dsc("gL", [128, NB, 24])
    o16 = dsc("o16", [16, 128, SQ])
    with contextlib.ExitStack() as st:
        C = Ctx(nc, st)
        cur = xT
        rb = W["rel_bias"]
        for l in range(n_layers):
            if l > 0:
                C.S.fresh_engine_sems(st, "L%d" % l)
                for b_ in C.b_P:
                    b_.w, b_.r = None, {}
            build_ffn(SQ, C, (cur, W["gn_ffn1"][l], W["ffn1_w1"][l], W["ffn1_w3"][l], W["ffn1_w2"][l], s1))
            build_mixin(SQ, C, (s1, W["gn_mix"][l], W["w_in"][l], W["qkg"][l], W["gbias"][l], fmT, vL, gL))
            build_dilated(SQ, 8, C, (fmT[0:8], fmT[8:16], vL[0:8], rb[:, 0:8], W["ohd"], W["exd"], W["ident"], W["J"],
                                     o16[0:8]))
            for g in range(2):
                build_nsa(SQ, C, (fmT[16 + 4 * g:20 + 4 * g], fmT[24 + g], fmT[26 + g], vL[8 + g], vL[10 + g],
                                  fmT[28 + g], fmT[30 + g], gL, W["posT"][l], W["cmp_k_w1"][l], W["cmp_k_w2"][l],
                                  W["cmp_v_w1"][l], W["cmp_v_w2"][l], W["kg"][l], rb[:, 8 + 4 * g:12 + 4 * g],
                                  W["ohw"], W["exw"], W["ohs"], W["exs"], W["ident"], W["J"], W["ovl"], W["mcmp"],
                                  W["rw"], W["vm"], W["am"], o16[8 + 4 * g:12 + 4 * g]), gcol0=12 * g)
            build_mixout(SQ, C, (s1, o16, W["gn_out"][l], W["w_out"][l], s2))
            nxt = yT if l == n_layers - 1 else s3
            build_ffn(SQ, C, (s2, W["gn_ffn2"][l], W["ffn2_w1"][l], W["ffn2_w3"][l], W["ffn2_w2"][l], nxt))
            cur = s3
        print("fused program: layers=%d ops=%d waits=%d" % (n_layers, C.S.ninst, C.S.nwaits))
    return nc


def kernel(x, ffn1_norm, ffn1_w1, ffn1_w3, ffn1_w2, mix_norm, w_in, gate_bias,
           q_norm_a, k_norm_a, q_norm_nsa, k_norm_nsa, cmp_pos, cmp_k_w1, cmp_k_w2,
           cmp_v_w1, cmp_v_w2, out_norm, w_out, ffn2_norm, ffn2_w1, ffn2_w3, ffn2_w2, rel_bias,
           n_layers=DEPTH, ncores=NCORES):
    f32 = lambda a: np.ascontiguousarray(np.asarray(a, dtype=np.float32))
    x = f32(x)
    B, S_, D = x.shape
    nc = _prog("fused", build_fused, n_layers, S_)
    cv = lambda a: np.ascontiguousarray(np.stack([_chunk_vec(a[l]) for l in range(DEPTH)], axis=0))
    ohd, exd, _ = toeplitz_consts("dil")
    ohw, exw, _ = toeplitz_consts("win")
    ohs, exs, _ = toeplitz_consts("sel")
    ident, J = host_consts()
    ovl, mcmp, rw, vm, am = nsa_consts(S_)
    shared = {
        "ffn1_w1": f32(ffn1_w1), "ffn1_w3": f32(ffn1_w3), "ffn1_w2": f32(ffn1_w2),
        "ffn2_w1": f32(ffn2_w1), "ffn2_w3": f32(ffn2_w3), "ffn2_w2": f32(ffn2_w2),
        "w_in": f32(w_in), "w_out": f32(w_out),
        "cmp_k_w1": f32(cmp_k_w1), "cmp_k_w2": f32(cmp_k_w2), "cmp_v_w1": f32(cmp_v_w1), "cmp_v_w2": f32(cmp_v_w2),
        "rel_bias": f32(rel_bias),
        "gn_ffn1": cv(ffn1_norm), "gn_mix": cv(mix_norm), "gn_out": cv(out_norm), "gn_ffn2": cv(ffn2_norm),
        "qkg": np.ascontiguousarray(np.stack([f32(q_norm_a), f32(k_norm_a), f32(q_norm_nsa), f32(k_norm_nsa)], axis=2)),
        "gbias": f32(gate_bias).reshape(DEPTH, 1, 24),
        "posT": np.ascontiguousarray(f32(cmp_pos).transpose(0, 2, 1)),
        "kg": f32(k_norm_nsa).reshape(DEPTH, 128, 1),
        "ohd": ohd, "exd": exd, "ohw": ohw, "exw": exw, "ohs": ohs, "exs": exs, "ident": ident, "J": J,
        "ovl": ovl, "mcmp": mcmp, "rw": rw, "vm": vm, "am": am,
    }
    in_maps = []
    for c in range(ncores):
        m = dict(shared)
        m["xT"] = np.ascontiguousarray(x[c % B].T)
        in_maps.append(m)
    res = run_bass_kernel_spmd(nc, in_maps, core_ids=list(range(ncores))).results
    out = np.zeros((B, S_, D), np.float32)
    for b in range(min(B, ncores)):
        out[b] = res[b]["yT"].T
    return out


kernel_fused = kernel
kernel = kernel_unfused
```
